# Optimizing a Trainium2 kernel written in Bass

```python
import jax
import jax.numpy as jnp
from jax import lax
import numpy as np


D_MODEL = 1024
BATCH = 16
SEQ = 2048
DEPTH = 4

GRID_W = 64
CTX_LEN = 256
N_MIXERS = 3
ALPHA = (2.0 * DEPTH) ** 0.25
BETA = (8.0 * DEPTH) ** -0.25
LN_EPS = 1e-5
RMS_EPS = 1e-6
N_MOD = 6

CONV_W = 31
D_GLU = 2 * D_MODEL
SHORT_W = 3
MLA_HEADS = 8
QK_NOPE = 128
QK_ROPE = 64
V_HEAD = 128
Q_RANK = 384
KV_RANK = 256
ROPE_THETA = 10000.0
ATTN_SCALE = (QK_NOPE + QK_ROPE) ** -0.5
Q_BLOCK = 128
N_EXPERTS = 64
TOP_K = 8
N_GROUPS = 8
TOPK_GROUPS = 4
D_EXPERT = D_MODEL // 4
D_SHARED = D_MODEL // 4
ROUTED_SCALE = 2.5
MOE_BLOCK = 128

N_A = (DEPTH + 2) // 3
N_B = (DEPTH + 1) // 3
N_C = DEPTH // 3

kernel_name = 'hybrid_dit_conformer_shortconv_mla_moe'


def layer_norm(x, g, b):
    xf = x.astype(jnp.float32)
    mu = jnp.mean(xf, -1, keepdims=True)
    var = jnp.mean(jnp.square(xf - mu), -1, keepdims=True)
    return ((xf - mu) * lax.rsqrt(var + LN_EPS)).astype(x.dtype) * g + b


def rms_norm(x, g):
    xf = x.astype(jnp.float32)
    return (xf * lax.rsqrt(jnp.mean(xf * xf, -1, keepdims=True) + RMS_EPS)).astype(x.dtype) * g


def depthwise_conv(u, w):
    k = w.shape[0]
    p = (k - 1) // 2
    return lax.conv_general_dilated(
        u, w[:, None, :].astype(u.dtype), window_strides=(1,), padding=[(p, p)],
        dimension_numbers=('NWC', 'WIO', 'NWC'), feature_group_count=u.shape[-1])


def swiglu(t, w1, w3, w2):
    return (jax.nn.silu(t @ w1) * (t @ w3)) @ w2


def axial_rope_tables(rows):
    n_freq = QK_ROPE // 4
    inv_freq = ROPE_THETA ** (-jnp.arange(n_freq, dtype=jnp.float32) / n_freq)
    r = jnp.broadcast_to(jnp.arange(rows, dtype=jnp.float32)[:, None], (rows, GRID_W)).reshape(-1)
    col = jnp.broadcast_to(jnp.arange(GRID_W, dtype=jnp.float32)[None, :], (rows, GRID_W)).reshape(-1)
    ang = jnp.concatenate([r[:, None] * inv_freq, col[:, None] * inv_freq], -1)
    return jnp.cos(ang), jnp.sin(ang)


def apply_rope(t, cos, sin):
    t1, t2 = jnp.split(t, 2, axis=-1)
    return jnp.concatenate([t1 * cos - t2 * sin, t1 * sin + t2 * cos], -1).astype(t.dtype)


def conformer_conv(h, w1, b1, dw, dwb, ng, nb, w2, b2):
    u = h @ w1 + b1
    u = u[..., :D_MODEL] * jax.nn.sigmoid(u[..., D_MODEL:])
    u = depthwise_conv(u, dw) + dwb
    u = jax.nn.silu(layer_norm(u, ng, nb))
    return u @ w2 + b2


def short_conv(h, w_in, dw, w_out):
    gb, gc, v = jnp.split(h @ w_in, 3, axis=-1)
    u = depthwise_conv(gc * v, dw)
    return (gb * u) @ w_out


def mla_down(t, w_dqkv, q_g, kv_g):
    d = t @ w_dqkv
    cq = rms_norm(d[..., :Q_RANK], q_g)
    ckv = rms_norm(d[..., Q_RANK:Q_RANK + KV_RANK], kv_g)
    k_pe = d[..., Q_RANK + KV_RANK:]
    return cq, ckv, k_pe


def mla_queries(cq, w_uq):
    b, l, _ = cq.shape
    q = (cq @ w_uq).reshape(b, l, MLA_HEADS, QK_NOPE + QK_ROPE)
    return q[..., :QK_NOPE], q[..., QK_NOPE:]


def mla_keys_values(ckv, w_uk, w_uv):
    b, l, _ = ckv.shape
    k_nope = (ckv @ w_uk).reshape(b, l, MLA_HEADS, QK_NOPE)
    v = (ckv @ w_uv).reshape(b, l, MLA_HEADS, V_HEAD)
    return k_nope, v


def mla_attend(qn, qp, kn, kp, v):
    s = jnp.einsum('bqhn,bkhn->bhqk', qn, kn) + jnp.einsum('bqhr,bkr->bhqk', qp, kp)
    p = jax.nn.softmax(s.astype(jnp.float32) * ATTN_SCALE, axis=-1).astype(v.dtype)
    return jnp.einsum('bhqk,bkhv->bqhv', p, v)


def mla_mixer(h, hc, cos, sin, w_dqkv, q_g, kv_g, w_uq, w_uk, w_uv, w_o, need_ctx):
    b, s, _ = h.shape
    cq, ckv, kp = mla_down(h, w_dqkv, q_g, kv_g)
    qn, qp = mla_queries(cq, w_uq)
    kn, v = mla_keys_values(ckv, w_uk, w_uv)
    qp = apply_rope(qp, cos[None, :, None, :], sin[None, :, None, :])
    kp = apply_rope(kp, cos[None], sin[None])
    cq_c, ckv_c, kp_c = mla_down(hc, w_dqkv, q_g, kv_g)
    kn_c, v_c = mla_keys_values(ckv_c, w_uk, w_uv)
    kn_all = jnp.concatenate([kn, kn_c], axis=1)
    kp_all = jnp.concatenate([kp, kp_c], axis=1)
    v_all = jnp.concatenate([v, v_c], axis=1)
    nb = s // Q_BLOCK

    def blocks(t):
        return jnp.moveaxis(t.reshape(b, nb, Q_BLOCK, *t.shape[2:]), 1, 0)

    o = lax.map(lambda qs: mla_attend(qs[0], qs[1], kn_all, kp_all, v_all), (blocks(qn), blocks(qp)))
    o = jnp.moveaxis(o, 0, 1).reshape(b, s, MLA_HEADS * V_HEAD)
    y = o @ w_o
    if need_ctx:
        qn_c, qp_c = mla_queries(cq_c, w_uq)
        oc = mla_attend(qn_c, qp_c, kn_c, kp_c, v_c)
        yc = oc.reshape(b, hc.shape[1], MLA_HEADS * V_HEAD) @ w_o
    else:
        yc = None
    return y, yc


def expert_dispatch(h, topi, wts, w1, w3, w2):
    t, d = h.shape
    a = t * TOP_K
    e_flat = topi.reshape(a)
    order = jnp.argsort(e_flat)
    e_sorted = e_flat[order]
    tok_sorted = (order // TOP_K).astype(jnp.int32)
    gate_sorted = wts.reshape(a)[order]
    counts = jnp.bincount(e_flat, length=N_EXPERTS)
    padded = (counts + MOE_BLOCK - 1) // MOE_BLOCK * MOE_BLOCK
    pad_end = jnp.cumsum(padded)
    pad_start = pad_end - padded
    start = jnp.cumsum(counts) - counts
    dest = pad_start[e_sorted] + (jnp.arange(a, dtype=jnp.int32) - start[e_sorted])
    n_blocks = -(-a // MOE_BLOCK) + N_EXPERTS
    p = n_blocks * MOE_BLOCK
    tok_buf = jnp.full((p,), t, jnp.int32).at[dest].set(tok_sorted)
    gate_buf = jnp.zeros((p,), h.dtype).at[dest].set(gate_sorted)
    block_start = jnp.arange(n_blocks, dtype=pad_end.dtype) * MOE_BLOCK
    block_expert = jnp.minimum(jnp.searchsorted(pad_end, block_start, side='right'), N_EXPERTS - 1)
    h_pad = jnp.concatenate([h, jnp.zeros((1, d), h.dtype)], axis=0)

    def run_block(args):
        toks, e = args
        return swiglu(h_pad[toks], w1[e], w3[e], w2[e])

    yb = lax.map(run_block, (tok_buf.reshape(n_blocks, MOE_BLOCK), block_expert))
    out = jnp.zeros((t + 1, d), h.dtype).at[tok_buf].add(yb.reshape(p, d) * gate_buf[:, None])
    return out[:t]


def moe(h, w_router, r_bias, w1, w3, w2, ws1, ws3, ws2):
    t = h.shape[0]
    s = jax.nn.sigmoid((h @ w_router).astype(jnp.float32))
    sel = s + r_bias.astype(jnp.float32)
    grp = sel.reshape(t, N_GROUPS, N_EXPERTS // N_GROUPS)
    gscore = jnp.sum(lax.top_k(grp, 2)[0], axis=-1)
    _, gidx = lax.top_k(gscore, TOPK_GROUPS)
    gmask = jnp.any(gidx[:, :, None] == jnp.arange(N_GROUPS)[None, None, :], axis=1)
    emask = jnp.repeat(gmask, N_EXPERTS // N_GROUPS, axis=1)
    _, topi = lax.top_k(jnp.where(emask, sel, -jnp.inf), TOP_K)
    w = jnp.take_along_axis(s, topi, axis=-1)
    w = (w / jnp.sum(w, -1, keepdims=True) * ROUTED_SCALE).astype(h.dtype)
    return expert_dispatch(h, topi, w, w1, w3, w2) + swiglu(h, ws1, ws3, ws2)


def setup_inputs(seed: int = 0) -> dict:
    key = jax.random.key(seed)
    ks = iter(jax.random.split(key, 40))
    D = D_MODEL

    def nrm(shape, scale):
        return jax.random.normal(next(ks), shape, jnp.float32) * scale

    def gain(shape):
        return 1.0 + nrm(shape, 0.02)

    return {
        'x': nrm((BATCH, SEQ, D), 1.0),
        'c': nrm((BATCH, D), 1.0),
        'ctx': nrm((BATCH, CTX_LEN, D), 1.0),
        'c_ctx': nrm((D,), 1.0),
        'ada_w': nrm((DEPTH, D, N_MOD * D), 0.5 * D ** -0.5),
        'ada_b': nrm((DEPTH, N_MOD * D), 0.02),
        'ln_g': gain((DEPTH, 2, D)),
        'ln_b': nrm((DEPTH, 2, D), 0.02),
        'conf_w1': nrm((N_A, D, D_GLU), D ** -0.5),
        'conf_b1': nrm((N_A, D_GLU), 0.02),
        'conf_dw': nrm((N_A, CONV_W, D), CONV_W ** -0.5),
        'conf_dwb': nrm((N_A, D), 0.02),
        'conf_ng': gain((N_A, D)),
        'conf_nb': nrm((N_A, D), 0.02),
        'conf_w2': nrm((N_A, D, D), BETA * D ** -0.5),
        'conf_b2': nrm((N_A, D), 0.02),
        'sc_w_in': nrm((N_B, D, 3 * D), D ** -0.5),
        'sc_dw': nrm((N_B, SHORT_W, D), SHORT_W ** -0.5),
        'sc_w_out': nrm((N_B, D, D), BETA * D ** -0.5),
        'mla_w_dqkv': nrm((N_C, D, Q_RANK + KV_RANK + QK_ROPE), D ** -0.5),
        'mla_q_g': gain((N_C, Q_RANK)),
        'mla_kv_g': gain((N_C, KV_RANK)),
        'mla_w_uq': nrm((N_C, Q_RANK, MLA_HEADS * (QK_NOPE + QK_ROPE)), Q_RANK ** -0.5),
        'mla_w_uk': nrm((N_C, KV_RANK, MLA_HEADS * QK_NOPE), KV_RANK ** -0.5),
        'mla_w_uv': nrm((N_C, KV_RANK, MLA_HEADS * V_HEAD), BETA * KV_RANK ** -0.5),
        'mla_w_o': nrm((N_C, MLA_HEADS * V_HEAD, D), BETA * (MLA_HEADS * V_HEAD) ** -0.5),
        'moe_router': nrm((DEPTH, D, N_EXPERTS), D ** -0.5),
        'moe_bias': nrm((DEPTH, N_EXPERTS), 0.01),
        'moe_w1': nrm((DEPTH, N_EXPERTS, D, D_EXPERT), D ** -0.5),
        'moe_w3': nrm((DEPTH, N_EXPERTS, D, D_EXPERT), D ** -0.5),
        'moe_w2': nrm((DEPTH, N_EXPERTS, D_EXPERT, D), BETA * D_EXPERT ** -0.5),
        'sh_w1': nrm((DEPTH, D, D_SHARED), D ** -0.5),
        'sh_w3': nrm((DEPTH, D, D_SHARED), D ** -0.5),
        'sh_w2': nrm((DEPTH, D_SHARED, D), BETA * D_SHARED ** -0.5),
    }


def reference(x, c, ctx, c_ctx, ada_w, ada_b, ln_g, ln_b,
              conf_w1, conf_b1, conf_dw, conf_dwb, conf_ng, conf_nb, conf_w2, conf_b2,
              sc_w_in, sc_dw, sc_w_out,
              mla_w_dqkv, mla_q_g, mla_kv_g, mla_w_uq, mla_w_uk, mla_w_uv, mla_w_o,
              moe_router, moe_bias, moe_w1, moe_w3, moe_w2, sh_w1, sh_w3, sh_w2):
    b, s, d = x.shape
    l_ctx = ctx.shape[1]
    rows = s // GRID_W
    cos, sin = axial_rope_tables(rows)
    attn_layers = [i for i in range(DEPTH) if i % N_MIXERS == 2]
    last_ctx_reader = attn_layers[-1] if attn_layers else -1

    for i in range(DEPTH):
        need_ctx = i < last_ctx_reader
        kind, j = i % N_MIXERS, i // N_MIXERS
        mod = jax.nn.silu(c) @ ada_w[i] + ada_b[i]
        mod_c = jax.nn.silu(c_ctx) @ ada_w[i] + ada_b[i]
        sh1, sc1, g1, sh2, sc2, g2 = jnp.split(mod[:, None, :], N_MOD, axis=-1)
        csh1, csc1, cg1, csh2, csc2, cg2 = jnp.split(mod_c, N_MOD, axis=-1)

        h = x * (1.0 + sc1) + sh1
        hc = ctx * (1.0 + csc1) + csh1
        if kind == 0:
            prm = (conf_w1[j], conf_b1[j], conf_dw[j], conf_dwb[j], conf_ng[j], conf_nb[j], conf_w2[j], conf_b2[j])
            y = conformer_conv(h, *prm)
            yc = conformer_conv(hc, *prm) if need_ctx else None
        elif kind == 1:
            prm = (sc_w_in[j], sc_dw[j], sc_w_out[j])
            y = short_conv(h, *prm)
            yc = short_conv(hc, *prm) if need_ctx else None
        else:
            y, yc = mla_mixer(h, hc, cos, sin, mla_w_dqkv[j], mla_q_g[j], mla_kv_g[j],
                              mla_w_uq[j], mla_w_uk[j], mla_w_uv[j], mla_w_o[j], need_ctx)
        x = layer_norm(ALPHA * x + g1 * y, ln_g[i, 0], ln_b[i, 0])
        if need_ctx:
            ctx = layer_norm(ALPHA * ctx + cg1 * yc, ln_g[i, 0], ln_b[i, 0])

        prm = (moe_router[i], moe_bias[i], moe_w1[i], moe_w3[i], moe_w2[i], sh_w1[i], sh_w3[i], sh_w2[i])
        h = (x * (1.0 + sc2) + sh2).reshape(b * s, d)
        if need_ctx:
            hc = (ctx * (1.0 + csc2) + csh2).reshape(b * l_ctx, d)
            out = moe(jnp.concatenate([h, hc], axis=0), *prm)
            y = out[:b * s].reshape(b, s, d)
            yc = out[b * s:].reshape(b, l_ctx, d)
            ctx = layer_norm(ALPHA * ctx + cg2 * yc, ln_g[i, 1], ln_b[i, 1])
        else:
            y = moe(h, *prm).reshape(b, s, d)
        x = layer_norm(ALPHA * x + g2 * y, ln_g[i, 1], ln_b[i, 1])
    return x
```

```python
import contextlib
import numpy as np
import concourse.bass as bass
import concourse.mybir as mybir
from concourse.bass_utils import run_bass_kernel_spmd

F32 = mybir.dt.float32
BF16 = mybir.dt.bfloat16
AF = mybir.ActivationFunctionType
ALU = mybir.AluOpType
AX = mybir.AxisListType

SAME_ENGINE_SYNC = True
SPARSE = True
DEPTH = 4
ALPHA = (2.0 * DEPTH) ** 0.25
LN_EPS = 1e-5
RMS_EPS = 1e-6
NTOK = 4608
SEQ = 2048
CTX = 256
BSTR = SEQ + CTX
NCORES = 8


class Tok:
    __slots__ = ("sem", "val", "eng")

    def __init__(self, sem, val, eng):
        self.sem, self.val, self.eng = sem, val, eng


class Res:
    __slots__ = ("w", "r", "name")

    def __init__(self, name=""):
        self.w = None
        self.r = {}
        self.name = name


class Prog:
    ENG = ("pe", "act", "dve", "pool", "sp")

    def __init__(self, nc, n_dma_sems=24):
        self.nc = nc
        self.eng = {"pe": nc.tensor, "act": nc.scalar, "dve": nc.vector,
                    "pool": nc.gpsimd, "sp": nc.sync}
        self.gstack = contextlib.ExitStack()
        self.esem = {}
        self.pending = {e: [] for e in self.ENG}
        self.last = {e: None for e in self.ENG}
        self.waited = {}
        self.nsem = 0
        self.dma_pool = []
        for i in range(n_dma_sems):
            h = self.gstack.enter_context(nc.semaphore("dq%d" % i))
            self.dma_pool.append([h, 0, None])
        self.dma_rr = 0
        self.new_epoch()
        self.pstack = None
        self.uid = 0

    def new_epoch(self):
        for e in self.ENG:
            assert not self.pending[e], "pending unsignaled ops on %s at epoch change" % e
            self.nsem += 1
            h = self.gstack.enter_context(self.nc.semaphore("e%s%d" % (e, self.nsem)))
            self.esem[e] = [h, 0]

    def _wait(self, e, tok, force=False):
        if tok is None:
            return
        if tok.eng == e and not (force or (SAME_ENGINE_SYNC and e != "pe")):
            return
        assert tok.sem is not None, "dependency on an unsignaled op (engine %s)" % tok.eng
        key = (e, id(tok.sem))
        if self.waited.get(key, -1) >= tok.val:
            return
        self.eng[e].wait_ge(tok.sem, tok.val)
        self.waited[key] = tok.val

    def _deps(self, e, R, W, force=False):
        for r in R:
            self._wait(e, r.w, force)
        for w in W:
            self._wait(e, w.w, force)
            for t in w.r.values():
                self._wait(e, t, force)

    def _update(self, tok, R, W, key):
        for w in W:
            w.w = tok
            w.r = {}
        for r in R:
            r.r[key] = tok

    def op(self, e, fn, R=(), W=(), sig=True):
        self._deps(e, R, W)
        ins = fn(self.eng[e])
        if sig:
            s = self.esem[e]
            s[1] += 1
            ins.then_inc(s[0], 1)
            tok = Tok(s[0], s[1], e)
            for p in self.pending[e]:
                p.sem, p.val = tok.sem, tok.val
            self.pending[e] = []
        else:
            tok = Tok(None, None, e)
            self.pending[e].append(tok)
        self.last[e] = tok
        self._update(tok, R, W, e)
        return tok

    def dma(self, q, out, in_, R=(), W=(), **kw):
        slot = self.dma_pool[self.dma_rr]
        self.dma_rr = (self.dma_rr + 1) % len(self.dma_pool)
        self._wait(q, slot[2], True)
        self._deps(q, R, W, True)
        ins = self.eng[q].dma_start(out=out, in_=in_, **kw)
        slot[1] += 16
        ins.then_inc(slot[0], 16)
        tok = Tok(slot[0], slot[1], None)
        slot[2] = tok
        self.uid += 1
        self._update(tok, R, W, "d%d" % self.uid)
        return tok

    def idma(self, out, out_off, in_, in_off, R=(), W=()):
        q = "pool"
        slot = self.dma_pool[self.dma_rr]
        self.dma_rr = (self.dma_rr + 1) % len(self.dma_pool)
        self._wait(q, slot[2], True)
        self._deps(q, R, W, True)
        ins = self.eng[q].indirect_dma_start(out=out, out_offset=out_off, in_=in_, in_offset=in_off)
        slot[1] += 16
        ins.then_inc(slot[0], 16)
        tok = Tok(slot[0], slot[1], None)
        slot[2] = tok
        self.uid += 1
        self._update(tok, R, W, "d%d" % self.uid)
        return tok

    def barrier(self):
        toks = []
        for e in self.ENG:
            assert not self.pending[e], "pending unsignaled ops on %s at barrier" % e
            if self.last[e] is not None:
                toks.append(self.last[e])
        for slot in self.dma_pool:
            if slot[2] is not None:
                toks.append(slot[2])
        for e in self.ENG:
            for t in toks:
                self._wait(e, t, True)

    def phase(self):
        self.pstack = contextlib.ExitStack()
        return self.pstack

    def sb(self, name, shape, dt=F32, glob=False):
        st = self.gstack if glob else self.pstack
        self.uid += 1
        return st.enter_context(self.nc.sbuf_tensor("%s_%d" % (name, self.uid), list(shape), dt))

    def ps(self, name, shape, dt=F32, glob=False):
        st = self.gstack if glob else self.pstack
        self.uid += 1
        return st.enter_context(self.nc.psum_tensor("%s_%d" % (name, self.uid), list(shape), dt))

    def tt(self, e, out, a, b, op, R, W):
        return self.op(e, lambda E: E.tensor_tensor(out=out, in0=a, in1=b, op=op), R, W)

    def ts(self, e, out, a, s1, s2, op0, op1, R, W):
        if s2 is None:
            return self.op(e, lambda E: E.tensor_scalar(out=out, in0=a, scalar1=s1, scalar2=None, op0=op0), R, W)
        return self.op(e, lambda E: E.tensor_scalar(out=out, in0=a, scalar1=s1, scalar2=s2, op0=op0, op1=op1), R, W)

    def actv(self, out, in_, func, R, W, bias=None, scale=None):
        kw = {}
        if bias is not None:
            kw["bias"] = bias
        if scale is not None:
            kw["scale"] = scale
        return self.op("act", lambda E: E.activation(out=out, in_=in_, func=func, **kw), R, W)

    def mm(self, out, lhsT, rhs, start, stop, R, W, sig=None):
        if sig is None:
            sig = stop
        return self.op("pe", lambda E: E.matmul(out, lhsT=lhsT, rhs=rhs, start=start, stop=stop), R, W, sig=sig)


def has_ctx(i):
    return i < 2


def active_runs(i):
    if has_ctx(i):
        return [(0, NTOK)]
    return [(0, SEQ), (BSTR, SEQ)]


def tile_row(t0):
    b, r = divmod(t0, BSTR)
    return 2 if r >= SEQ else b


class Consts:
    pass


class Epi:
    def __init__(self, P, Dm, C, layer, which, nxt, router, src, dst, hdst, psT, r_psT, pslog=None, r_pslog=None,
                 ns=4):
        self.ns = ns
        self.P, self.Dm, self.C = P, Dm, C
        self.layer, self.which, self.nxt, self.router = layer, which, nxt, router
        self.src, self.dst, self.hdst = src, dst, hdst
        self.psT, self.r_psT, self.pslog, self.r_pslog = psT, r_psT, pslog, r_pslog
        self.row = None
        self.q = []
        sb = P.sb
        self.G = sb("G", [128, 1024]); self.rG = Res()
        self.LG = sb("LG", [128, 1024]); self.LB = sb("LB", [128, 1024]); self.rL = Res()
        if nxt is not None:
            self.LGA = sb("LGA", [128, 1024]); self.LBAS = sb("LBAS", [128, 1024]); self.rAS = Res()
        self.xt = [sb("xt", [128, 1024]) for _ in range(ns)]; self.rxt = [Res() for _ in range(ns)]
        self.z = [sb("z", [128, 1024]) for _ in range(ns)]; self.rz = [Res() for _ in range(ns)]
        self.st = sb("st", [128, ns, 2, 6]); self.rst = [Res() for _ in range(ns)]
        self.mv = sb("mv", [128, ns, 4]); self.rmv = Res()
        if nxt is not None:
            self.hTb = sb("hTb", [128, 8, ns * 128], BF16); self.rhTb = Res()
            if router:
                self.nh32 = 2 if ns > 2 else 1
                self.hT32 = [sb("hT32", [128, 8, 128]) for _ in range(self.nh32)]; self.rhT32 = [Res(), Res()]
        lg = Dm["ln_g%d" % layer][which - 1:which, :]
        lb = Dm["ln_b%d" % layer][which - 1:which, :]
        P.dma("sp", self.LG[:], lg.broadcast_to([128, 1024]), W=[self.rL])
        P.dma("sp", self.LB[:], lb.broadcast_to([128, 1024]), W=[self.rL])
        if router:
            self.wr = sb("wr", [128, 8, 64]); self.rwr = Res()
            P.dma("sp", self.wr[:], Dm["moe_router%d" % layer].rearrange("(k p) e -> p k e", p=128), W=[self.rwr])
            self.rb = sb("rb", [128, ns, 64])
            for i in range(ns):
                P.dma("sp", self.rb[:, i, :], Dm["moe_bias%d" % layer].broadcast_to([128, 64]), W=[self.rwr])
            self.gt = {}
            for nm, w in (("s", 64), ("sel", 64), ("eq", 64), ("m1", 8), ("m2", 8), ("gs", 8),
                          ("t8", 8), ("gm", 8), ("pen", 8), ("msk", 64), ("t8e", 8),
                          ("den", 1), ("rden", 1)):
                self.gt[nm] = (sb("g_" + nm, [128, ns, w]), Res())
            self.gt["sel2"] = self.gt["eq"]
            for nm in ("selm", "gun", "gate"):
                self.gt[nm] = self.gt["msk"]

    def ensure_row(self, j):
        if self.row == j:
            return
        self.row = j
        P, Dm = self.P, self.Dm
        mr = Dm["modrow%d" % self.layer]
        off = 2048 if self.which == 1 else 5120
        P.dma("sp", self.G[:], mr[j:j + 1, off:off + 1024].broadcast_to([128, 1024]), W=[self.rG])
        if self.nxt is not None:
            nl, nw = self.nxt
            mr2 = Dm["modrow%d" % nl]
            so = 0 if nw == 1 else 3072
            P.dma("sp", self.LBAS[:], mr2[j:j + 1, so:so + 1024].broadcast_to([128, 1024]), W=[self.rAS])
            P.dma("sp", self.LGA[:], mr2[j:j + 1, so + 1024:so + 2048].broadcast_to([128, 1024]), W=[self.rAS])
            scr, rscr = self.xt[self.ns - 1], self.rxt[self.ns - 1]
            P.tt("pool", scr[:], self.LGA[:], self.LB[:], ALU.mult, [self.rAS, self.rL], [rscr])
            P.tt("pool", self.LBAS[:], self.LBAS[:], scr[:], ALU.add, [self.rAS, rscr], [self.rAS])
            P.tt("pool", self.LGA[:], self.LGA[:], self.LG[:], ALU.mult, [self.rAS, self.rL], [self.rAS])

    def tile(self, t0, ysrc):
        P = self.P
        j = tile_row(t0)
        if self.q and (len(self.q) == self.ns or j != self.row or t0 != self.q[-1] + 128):
            self.flush()
        self.ensure_row(j)
        i = len(self.q)
        self.q.append(t0)
        xt, rxt, z, rz = self.xt[i], self.rxt[i], self.z[i], self.rz[i]
        P.dma("sp", xt[:], self.src[t0:t0 + 128, :], W=[rxt])
        for hf in range(2):
            ap, res = ysrc[hf]
            P.tt("dve", z[:, hf * 512:(hf + 1) * 512], ap, self.G[:, hf * 512:(hf + 1) * 512], ALU.mult,
                 [res, self.rG], [rz])

    def flush(self):
        P, C = self.P, self.C
        q = self.q
        n = len(q)
        if n == 0:
            return
        self.q = []
        T0 = q[0]
        mv, rmv = self.mv, self.rmv
        for i in range(n):
            P.tt("dve", self.z[i][:], self.z[i][:], self.xt[i][:], ALU.add, [self.rz[i], self.rxt[i]], [self.rz[i]])
        for i in range(n):
            z, rz = self.z[i], self.rz[i]
            for hf in range(2):
                P.op("dve", lambda E, hf=hf, i=i, z=z: E.bn_stats(out=self.st[:, i, hf, :],
                                                              in_=z[:, hf * 512:(hf + 1) * 512]), [rz], [self.rst[i]])
            P.op("dve", lambda E, i=i: E.bn_aggr(out=mv[:, i, 0:2], in_=self.st[:, i, :, :].rearrange("p a b -> p (a b)")),
                 [self.rst[i]], [rmv])
        P.actv(mv[:, 0:n, 2:3], mv[:, 0:n, 1:2], AF.Sqrt, [rmv], [rmv], bias=C.eps_ln[:, 0:1], scale=1.0)
        P.op("dve", lambda E: E.reciprocal(out=mv[:, 0:n, 2:3], in_=mv[:, 0:n, 2:3]), [rmv], [rmv])
        P.op("dve", lambda E: E.scalar_tensor_tensor(out=mv[:, 0:n, 3:4], in0=mv[:, 0:n, 0:1], scalar=-1.0,
                                                     in1=mv[:, 0:n, 2:3], op0=ALU.mult, op1=ALU.mult), [rmv], [rmv])
        for i in range(n):
            z, rz = self.z[i], self.rz[i]
            P.actv(z[:], z[:], AF.Identity, [rz, rmv], [rz], bias=mv[:, i, 3:4], scale=mv[:, i, 2:3])
        if self.nxt is not None:
            for i in range(n):
                z, rz, h, rh = self.z[i], self.rz[i], self.xt[i], self.rxt[i]
                P.tt("dve", h[:], z[:], self.LGA[:], ALU.mult, [rz, self.rAS], [rh])
                P.tt("dve", h[:], h[:], self.LBAS[:], ALU.add, [rh, self.rAS], [rh])
        for i in range(n):
            z, rz = self.z[i], self.rz[i]
            P.tt("pool", z[:], z[:], self.LG[:], ALU.mult, [rz, self.rL], [rz])
            P.tt("pool", z[:], z[:], self.LB[:], ALU.add, [rz, self.rL], [rz])
            P.dma("sp", self.dst[q[i]:q[i] + 128, :], z[:], R=[rz])
        if self.nxt is None:
            return
        for i in range(n):
            h, rh = self.xt[i], self.rxt[i]
            for k in range(8):
                P.op("pe", lambda E, k=k, h=h: E.transpose(out=self.psT[k // 4][:, (k % 4) * 128:(k % 4 + 1) * 128],
                                                          in_=h[:, k * 128:(k + 1) * 128], identity=C.ident32[:]),
                     [rh, C.r_const], [self.r_psT[k // 4]], sig=(k % 4 == 3))
            hlo = self.hTb[:, 0:4, i * 128:(i + 1) * 128]
            hhi = self.hTb[:, 4:8, i * 128:(i + 1) * 128]
            p0 = self.psT[0][:].rearrange("p (k t) -> p k t", k=4)
            p1 = self.psT[1][:].rearrange("p (k t) -> p k t", k=4)
            if self.router:
                self.P.dma("pool", self.Dm["hrow"][q[i]:q[i] + 128, :], h[:], R=[rh])
                h32, rh32 = self.hT32[i % self.nh32], self.rhT32[i % self.nh32]
                P.op("act", lambda E, h32=h32, p0=p0: E.copy(out=h32[:, 0:4, :], in_=p0), [self.r_psT[0]], [rh32])
                P.op("act", lambda E, hlo=hlo, p0=p0: E.copy(out=hlo, in_=p0), [self.r_psT[0]], [self.rhTb])
                P.op("dve", lambda E, h32=h32, p1=p1: E.tensor_copy(out=h32[:, 4:8, :], in_=p1), [self.r_psT[1]], [rh32])
                P.op("dve", lambda E, hhi=hhi, p1=p1: E.tensor_copy(out=hhi, in_=p1), [self.r_psT[1]], [self.rhTb])
                for k in range(8):
                    P.mm(self.pslog[:, i * 64:(i + 1) * 64], h32[:, k, :], self.wr[:, k, :], k == 0, k == 7,
                         [rh32, self.rwr], [self.r_pslog])
            else:
                P.op("act", lambda E, hlo=hlo, p0=p0: E.copy(out=hlo, in_=p0), [self.r_psT[0]], [self.rhTb])
                P.op("dve", lambda E, hhi=hhi, p1=p1: E.tensor_copy(out=hhi, in_=p1), [self.r_psT[1]], [self.rhTb])
        P.dma("sp", self.hdst[:, :, T0:T0 + n * 128], self.hTb[:, :, 0:n * 128], R=[self.rhTb])
        if self.router:
            self.gating(T0, n)

    def gating(self, T0, n):
        P = self.P
        g = self.gt
        BIG = 1.0e4

        def T(nm):
            return g[nm][0][:, 0:n, :]

        def R(nm):
            return g[nm][1]

        def g3(ap):
            return ap.rearrange("p n (g e) -> p (n g) e", e=8)

        def f2(ap):
            return ap.rearrange("p n w -> p (n w)")
        pl = self.pslog[:, 0:n * 64].rearrange("p (n e) -> p n e", e=64)
        P.actv(T("s"), pl, AF.Sigmoid, [self.r_pslog], [R("s")])
        P.tt("dve", T("sel"), T("s"), self.rb[:, 0:n, :], ALU.add, [R("s"), self.rwr], [R("sel")])
        P.op("dve", lambda E: E.tensor_reduce(out=f2(T("m1")), in_=g3(T("sel")), axis=AX.X, op=ALU.max), [R("sel")], [R("m1")])
        m1b = f2(T("m1")).unsqueeze(2).broadcast_to([128, n * 8, 8])
        P.tt("dve", g3(T("eq")), g3(T("sel")), m1b, ALU.is_equal, [R("sel"), R("m1")], [R("eq")])
        P.op("dve", lambda E: E.scalar_tensor_tensor(out=f2(T("sel2")), in0=f2(T("eq")), scalar=-BIG, in1=f2(T("sel")),
                                                     op0=ALU.mult, op1=ALU.add), [R("eq"), R("sel")], [R("sel2")])
        P.op("dve", lambda E: E.tensor_reduce(out=f2(T("m2")), in_=g3(T("sel2")), axis=AX.X, op=ALU.max), [R("sel2")], [R("m2")])
        P.tt("dve", T("gs"), T("m1"), T("m2"), ALU.add, [R("m1"), R("m2")], [R("gs")])
        for i in range(n):
            P.op("dve", lambda E, i=i: E.max(out=g["t8"][0][:, i, :], in_=g["gs"][0][:, i, :]), [R("gs")], [R("t8")])
        thr = g["t8"][0][:, 0:n, 3:4].broadcast_to([128, n, 8])
        P.tt("dve", T("gm"), T("gs"), thr, ALU.is_ge, [R("gs"), R("t8")], [R("gm")])
        P.ts("dve", f2(T("pen")), f2(T("gm")), -1.0, BIG, ALU.add, ALU.mult, [R("gm")], [R("pen")])
        gmb = f2(T("gm")).unsqueeze(2).broadcast_to([128, n * 8, 8])
        penb = f2(T("pen")).unsqueeze(2).broadcast_to([128, n * 8, 8])
        P.tt("dve", g3(T("msk")), g3(T("sel")), gmb, ALU.mult, [R("sel"), R("gm")], [R("msk")])
        P.tt("dve", g3(T("msk")), g3(T("msk")), penb, ALU.add, [R("msk"), R("pen")], [R("msk")])
        for i in range(n):
            P.op("dve", lambda E, i=i: E.max(out=g["t8e"][0][:, i, :], in_=g["msk"][0][:, i, :]), [R("msk")], [R("t8e")])
        thr8 = g["t8e"][0][:, 0:n, 7:8].broadcast_to([128, n, 64])
        P.tt("dve", T("selm"), T("msk"), thr8, ALU.is_ge, [R("msk"), R("t8e")], [R("selm")])
        P.tt("dve", T("gun"), T("s"), T("selm"), ALU.mult, [R("s"), R("selm")], [R("gun")])
        P.op("dve", lambda E: E.tensor_reduce(out=f2(T("den")), in_=T("gun"), axis=AX.X, op=ALU.add), [R("gun")], [R("den")])
        P.op("dve", lambda E: E.reciprocal(out=T("rden"), in_=T("den")), [R("den")], [R("rden")])
        rdb = g["rden"][0][:, 0:n, 0:1].broadcast_to([128, n, 64])
        P.op("dve", lambda E: E.scalar_tensor_tensor(out=T("gate"), in0=T("gun"), scalar=2.5, in1=rdb,
                                                     op0=ALU.mult, op1=ALU.mult), [R("gun"), R("rden")], [R("gate")])
        P.dma("sp", self.Dm["gates"][T0:T0 + n * 128, :].rearrange("(n p) e -> p n e", p=128), T("gate"), R=[R("gate")])


def phase_consts(P, C):
    C.r_const = Res()
    C.ident32 = P.sb("ident32", [128, 128], F32, glob=True)
    C.identb = P.sb("identb", [128, 128], BF16, glob=True)
    C.ones_avg = P.sb("ones_avg", [128, 128], F32, glob=True)
    P.op("pool", lambda e: e.memset(C.ident32[:], 1.0), W=[C.r_const])
    P.op("pool", lambda e: e.affine_select(out=C.ident32[:], in_=C.ident32[:], pattern=[[-1, 128]],
                                           compare_op=ALU.is_equal, fill=0.0, base=0, channel_multiplier=1),
         R=[C.r_const], W=[C.r_const])
    P.op("pool", lambda e: e.tensor_copy(out=C.identb[:], in_=C.ident32[:]), R=[C.r_const], W=[C.r_const])
    P.op("pool", lambda e: e.memset(C.ones_avg[:], 1.0 / 1024.0), W=[C.r_const])
    C.eps_ln = P.sb("eps_ln", [128, 4], F32, glob=True)
    P.op("pool", lambda e: e.memset(C.eps_ln[:, 0:1], LN_EPS / (ALPHA * ALPHA)), W=[C.r_const])
    P.op("pool", lambda e: e.memset(C.eps_ln[:, 1:2], LN_EPS), W=[C.r_const])
    P.op("pool", lambda e: e.memset(C.eps_ln[:, 2:3], RMS_EPS), W=[C.r_const])


def phase_mod(P, Dm, C, layers):
    with P.phase():
        ccT = P.sb("ccT", [128, 8, 4]); r_cc = Res()
        sT = P.sb("sT", [128, 8, 4]); r_sT = Res()
        P.dma("sp", ccT[:].rearrange("p k j -> p (k j)"), Dm["ccT"], W=[r_cc])
        P.actv(sT[:], ccT[:], AF.Silu, [r_cc], [r_sT])
        wt = [P.sb("adaw", [128, 8, 512]) for _ in range(2)]; r_wt = [Res(), Res()]
        adab = P.sb("adab", [4, 6144]); r_ab = Res()
        msb = P.sb("msb", [4, 6144]); r_ms = Res()
        ps = [P.ps("psm", [128, 512]) for _ in range(2)]; r_ps = [Res(), Res()]
        n = 0
        for i in layers:
            P.dma("sp", adab[:], Dm["ada_b%d" % i].broadcast_to([4, 6144]), W=[r_ab])
            for nt in range(12):
                b = n % 2
                n += 1
                P.dma("sp" if nt % 2 == 0 else "act", wt[b][:],
                      Dm["ada_w%d" % i][:, nt * 512:(nt + 1) * 512].rearrange("(k p) f -> p k f", p=128), W=[r_wt[b]])
                for k in range(8):
                    P.mm(ps[b][0:4, :], sT[:, k, :], wt[b][:, k, :], k == 0, k == 7, [r_sT, r_wt[b]], [r_ps[b]])
                P.tt("dve", msb[:, nt * 512:(nt + 1) * 512], ps[b][0:4, :], adab[:, nt * 512:(nt + 1) * 512],
                     ALU.add, [r_ps[b], r_ab], [r_ms])
            for off in (1024, 4096):
                P.ts("dve", msb[:, off:off + 1024], msb[:, off:off + 1024], 1.0, None, ALU.add, None, [r_ms], [r_ms])
            for off in (2048, 5120):
                P.ts("dve", msb[:, off:off + 1024], msb[:, off:off + 1024], 1.0 / ALPHA, None, ALU.mult, None,
                     [r_ms], [r_ms])
            P.dma("sp", Dm["modrow%d" % i], msb[:], R=[r_ms])
        P.barrier()


def phase_prologue(P, Dm, C, layer):
    with P.phase():
        A = P.sb("A", [128, 1024]); S = P.sb("S", [128, 1024]); rAS = Res()
        xt = [P.sb("xt", [128, 1024]) for _ in range(2)]; rxt = [Res(), Res()]
        h = [P.sb("h", [128, 1024]) for _ in range(2)]; rh = [Res(), Res()]
        hTb = [P.sb("hTb", [128, 8, 128], BF16) for _ in range(2)]; rhTb = [Res(), Res()]
        psT = [P.ps("psT", [128, 512]) for _ in range(2)]; r_psT = [Res(), Res()]
        mr = Dm["modrow%d" % layer]
        row = None
        n = 0
        runs = [(0, NTOK)] if layer <= 2 else active_runs(layer)
        for (s0, ln) in runs:
            for t0 in range(s0, s0 + ln, 128):
                j = tile_row(t0)
                if j != row:
                    row = j
                    P.dma("sp", S[:], mr[j:j + 1, 0:1024].broadcast_to([128, 1024]), W=[rAS])
                    P.dma("sp", A[:], mr[j:j + 1, 1024:2048].broadcast_to([128, 1024]), W=[rAS])
                b = n % 2
                n += 1
                P.dma("sp", xt[b][:], Dm["xin"][t0:t0 + 128, :], W=[rxt[b]])
                P.tt("pool", h[b][:], xt[b][:], A[:], ALU.mult, [rxt[b], rAS], [rh[b]])
                P.tt("dve", h[b][:], h[b][:], S[:], ALU.add, [rh[b], rAS], [rh[b]])
                for k in range(8):
                    P.op("pe", lambda E, k=k, b=b: E.transpose(out=psT[k // 4][:, (k % 4) * 128:(k % 4 + 1) * 128],
                                                               in_=h[b][:, k * 128:(k + 1) * 128], identity=C.ident32[:]),
                         [rh[b], C.r_const], [r_psT[k // 4]], sig=(k % 4 == 3))
                P.op("act", lambda E, b=b: E.copy(out=hTb[b][:, 0:4, :].rearrange("p k t -> p (k t)"), in_=psT[0][:]),
                     [r_psT[0]], [rhTb[b]])
                P.op("dve", lambda E, b=b: E.tensor_copy(out=hTb[b][:, 4:8, :].rearrange("p k t -> p (k t)"),
                                                         in_=psT[1][:]), [r_psT[1]], [rhTb[b]])
                P.dma("sp", Dm["hA"][:, :, t0:t0 + 128], hTb[b][:], R=[rhTb[b]])
        P.barrier()


def phase_convmix(P, Dm, C, layer, src, dst, nxt, bg):
    kind, j = layer % 3, layer // 3
    KW = 31 if kind == 0 else 3
    PAD = (KW - 1) // 2
    ctx_on = has_ctx(layer)
    NW1 = 2048 if kind == 0 else 3072
    w1name = "conf_w1_%d" % layer if kind == 0 else "sc_w_in_%d" % layer
    w2name = "conf_w2_%d" % layer if kind == 0 else "sc_w_out_%d" % layer
    dwname = "conf_dwT_%d" % layer if kind == 0 else "sc_dwT_%d" % layer
    VLEN = (SEQ + 2 * PAD) + (CTX + 2 * PAD)
    layer_stack = contextlib.ExitStack()
    P.uid += 1
    diag = layer_stack.enter_context(P.nc.sbuf_tensor("diag_%d" % P.uid, [128, 8 * KW, 128], BF16)); r_diag = Res()
    dwT = layer_stack.enter_context(P.nc.sbuf_tensor("dwT_%d" % P.uid, [128, 8, KW], F32)); r_dw = Res()
    P.dma("sp", dwT[:].rearrange("p c k -> p (c k)"), Dm[dwname], W=[r_dw])
    for c in range(8):
        for k in range(KW):
            P.ts("dve", diag[:, c * KW + k, :], C.identb[:], dwT[:, c, k:k + 1], None, ALU.mult, None,
                 [C.r_const, r_dw], [r_diag])
    for b in range(2):
        seqs = [(b * BSTR, SEQ, 0)]
        if ctx_on:
            seqs.append((b * BSTR + SEQ, CTX, SEQ + 2 * PAD))
        outer = contextlib.ExitStack()
        with outer:
            P.uid += 1
            vT = outer.enter_context(P.nc.sbuf_tensor("vT_%d" % P.uid, [128, 8, VLEN], BF16)); r_vT = Res()
            gbT = None
            if kind == 1:
                P.uid += 1
                gbT = outer.enter_context(P.nc.sbuf_tensor("gbT_%d" % P.uid, [128, 8, BSTR], BF16)); r_gbT = Res()
            with P.phase():
                bg.attach(3)
                P.op("pool", lambda E: E.memset(vT[:], 0.0), W=[r_vT])
                w1 = P.sb("w1", [128, 8, NW1], BF16); r_w1 = Res()
                for k in range(8):
                    P.dma("pool", w1[:, k, :], Dm[w1name][k * 128:(k + 1) * 128, :], W=[r_w1])
                if kind == 0:
                    b1T = P.sb("b1T", [128, 16]); r_b1 = Res()
                    P.dma("sp", b1T[:], Dm["conf_b1T_%d" % layer], W=[r_b1])
                hT = [P.sb("hT", [128, 8, 512], BF16) for _ in range(2)]; r_hT = [Res(), Res()]
                sg = [P.sb("sg", [128, 512]) for _ in range(2)]; r_sg = [Res(), Res()]
                NB = 3 if kind == 1 else 2
                psa = [[P.ps("psa", [128, 512]) for _ in range(NB)] for _ in range(2)]
                r_psa = [[Res() for _ in range(NB)] for _ in range(2)]
                n = 0
                m = 0
                for (s0, ln, voff) in seqs:
                    for t0 in range(0, ln, 512):
                        N = min(512, ln - t0)
                        hb = n % 2
                        n += 1
                        P.dma("sp", hT[hb][:, :, 0:N], Dm["hA"][:, :, s0 + t0:s0 + t0 + N], W=[r_hT[hb]])
                        for fc in range(8):
                            bg.step(3)
                            pb = m % 2
                            m += 1
                            for w in range(NB):
                                col = w * 1024 + fc * 128
                                for k in range(8):
                                    P.mm(psa[pb][w][:, 0:N], w1[:, k, col:col + 128], hT[hb][:, k, 0:N], k == 0, k == 7,
                                         [r_w1, r_hT[hb]], [r_psa[pb][w]])
                            vdst = vT[:, fc, voff + PAD + t0:voff + PAD + t0 + N]
                            if kind == 0:
                                P.actv(sg[pb][:, 0:N], psa[pb][1][:, 0:N], AF.Sigmoid, [r_psa[pb][1], r_b1], [r_sg[pb]],
                                       bias=b1T[:, 8 + fc:9 + fc])
                                P.op("dve", lambda E, pb=pb, N=N, fc=fc, vdst=vdst: E.scalar_tensor_tensor(
                                    out=vdst, in0=psa[pb][0][:, 0:N], scalar=b1T[:, fc:fc + 1], in1=sg[pb][:, 0:N],
                                    op0=ALU.add, op1=ALU.mult), [r_psa[pb][0], r_sg[pb], r_b1], [r_vT])
                            else:
                                P.op("act", lambda E, pb=pb, N=N, fc=fc, s0=s0, t0=t0: E.copy(
                                    out=gbT[:, fc, s0 - b * BSTR + t0:s0 - b * BSTR + t0 + N], in_=psa[pb][0][:, 0:N]),
                                    [r_psa[pb][0]], [r_gbT])
                                P.op("act", lambda E, pb=pb, N=N: E.copy(out=sg[pb][:, 0:N], in_=psa[pb][1][:, 0:N]),
                                     [r_psa[pb][1]], [r_sg[pb]])
                                P.tt("dve", vdst, psa[pb][2][:, 0:N], sg[pb][:, 0:N], ALU.mult,
                                     [r_psa[pb][2], r_sg[pb]], [r_vT])
                bg.detach()
                P.barrier()
            with P.phase():
                w2 = P.sb("w2", [128, 8, 1024], BF16); r_w2 = Res()
                for k in range(8):
                    P.dma("pool", w2[:, k, :], Dm[w2name][k * 128:(k + 1) * 128, :], W=[r_w2])
                sT = [P.sb("sT", [128, 8, 512], BF16) for _ in range(1)]; r_sT = [Res(), Res()]
                psc = [P.ps("psc", [128, 512]) for _ in range(2)]; r_psc = [Res(), Res()]
                psy = [P.ps("psy", [128, 512]) for _ in range(2)]; r_psy = [Res(), Res()]
                psT = [P.ps("psT", [128, 512]) for _ in range(2)]; r_psT = [Res(), Res()]
                if kind == 0:
                    pvec = P.sb("pvec", [128, 3, 8]); r_pv = Res()
                    P.dma("sp", pvec[:].rearrange("p a c -> p (a c)"), Dm["conf_pvec_%d" % layer], W=[r_pv])
                    cv = P.sb("cv", [128, 8, 512]); r_cvc = [Res() for _ in range(8)]
                    sq = [P.sb("sq", [128, 512])] * 2; r_sq = [Res()] * 2
                    pss = [P.ps("pss", [128, 512]) for _ in range(2)]; r_pss = Res()
                    meanB = P.sb("meanB", [128, 512]); rstdB = P.sb("rstdB", [128, 512]); r_mr = Res()
                    pslog, r_pslog = pss[0], r_pss
                    r_b2 = Res()
                    b2f = cv[0:1, 0:2, :].rearrange("p a b -> p (a b)")
                    b2t = cv[0:1, 2:4, :].rearrange("p a b -> p (a b)")
                    rsc = [r_cvc[0], r_cvc[1], r_cvc[2], r_cvc[3]]
                    b2hl = P.sb("b2hl", [1, 2, 1024], BF16)
                    ones1 = P.sb("ones1", [1, 128], BF16)
                    P.op("pool", lambda E: E.memset(ones1[:], 1.0), W=[r_b2])
                    P.dma("sp", b2f, Dm["conf_b2_%d" % layer], W=rsc)
                    P.op("pool", lambda E: E.tensor_copy(out=b2hl[:, 0, :], in_=b2f), rsc, [r_b2])
                    P.op("pool", lambda E: E.tensor_copy(out=b2t, in_=b2hl[:, 0, :]), [r_b2], rsc)
                    P.tt("pool", b2t, b2f, b2t, ALU.subtract, rsc, rsc)
                    P.op("pool", lambda E: E.tensor_copy(out=b2hl[:, 1, :], in_=b2t), rsc, [r_b2])
                else:
                    pslog = P.ps("pslog", [128, 512]); r_pslog = Res()
                epi = Epi(P, Dm, C, layer, 1, nxt, True, src, dst, Dm["hB"], psT, r_psT, pslog, r_pslog,
                          ns=(2 if kind == 0 else 4))
                n = 0
                m = 0
                for (s0, ln, voff) in seqs:
                    for t0 in range(0, ln, 512):
                        N = min(512, ln - t0)
                        sb_ = 0
                        n += 1
                        for c in range(8):
                            pb = m % 2
                            m += 1
                            for k in range(KW):
                                P.mm(psc[pb][:, 0:N], diag[:, c * KW + k, :],
                                     vT[:, c, voff + t0 + k:voff + t0 + k + N], k == 0, k == KW - 1,
                                     [r_diag, r_vT], [r_psc[pb]])
                            if kind == 0:
                                P.actv(cv[:, c, 0:N], psc[pb][:, 0:N], AF.Identity, [r_psc[pb], r_pv], [r_cvc[c]],
                                       bias=pvec[:, 0, c:c + 1])
                                P.actv(sq[pb][:, 0:N], cv[:, c, 0:N], AF.Square, [r_cvc[c]], [r_sq[pb]])
                                P.mm(pss[0][:, 0:N], C.ones_avg[:], cv[:, c, 0:N], c == 0, c == 7,
                                     [C.r_const, r_cvc[c]], [r_pss], sig=True)
                                P.mm(pss[1][:, 0:N], C.ones_avg[:], sq[pb][:, 0:N], c == 0, c == 7,
                                     [C.r_const, r_sq[pb]], [r_pss], sig=True)
                            else:
                                P.tt("dve", sT[sb_][:, c, 0:N], psc[pb][:, 0:N],
                                     gbT[:, c, s0 - b * BSTR + t0:s0 - b * BSTR + t0 + N], ALU.mult,
                                     [r_psc[pb], r_gbT], [r_sT[sb_]])
                        if kind == 0:
                            P.op("act", lambda E, N=N: E.copy(out=meanB[:, 0:N], in_=pss[0][:, 0:N]), [r_pss], [r_mr])
                            P.actv(rstdB[:, 0:N], pss[0][:, 0:N], AF.Square, [r_pss], [r_mr])
                            P.tt("dve", rstdB[:, 0:N], pss[1][:, 0:N], rstdB[:, 0:N], ALU.subtract, [r_pss, r_mr], [r_mr])
                            P.actv(rstdB[:, 0:N], rstdB[:, 0:N], AF.Sqrt, [r_mr], [r_mr], bias=C.eps_ln[:, 1:2], scale=1.0)
                            P.op("dve", lambda E, N=N: E.reciprocal(out=rstdB[:, 0:N], in_=rstdB[:, 0:N]), [r_mr], [r_mr])
                            for c in range(8):
                                P.tt("pool", cv[:, c, 0:N], cv[:, c, 0:N], meanB[:, 0:N], ALU.subtract,
                                     [r_cvc[c], r_mr], [r_cvc[c]])
                                P.tt("dve", cv[:, c, 0:N], cv[:, c, 0:N], rstdB[:, 0:N], ALU.mult,
                                     [r_cvc[c], r_mr], [r_cvc[c]])
                                P.actv(sT[sb_][:, c, 0:N], cv[:, c, 0:N], AF.Silu, [r_cvc[c], r_pv], [r_sT[sb_]],
                                       bias=pvec[:, 2, c:c + 1], scale=pvec[:, 1, c:c + 1])
                        for jt in range(N // 128):
                            for hf in range(2):
                                for k in range(8):
                                    P.mm(psy[hf][:, :], sT[sb_][:, k, jt * 128:(jt + 1) * 128],
                                         w2[:, k, hf * 512:(hf + 1) * 512], k == 0, (k == 7 and kind != 0),
                                         [r_sT[sb_], r_w2], [r_psy[hf]])
                                if kind == 0:
                                    for hl in range(2):
                                        P.mm(psy[hf][:, :], ones1[:], b2hl[:, hl, hf * 512:(hf + 1) * 512], False, hl == 1,
                                             [r_b2], [r_psy[hf]])
                            epi.tile(s0 + t0 + jt * 128, [(psy[0][:, :], r_psy[0]), (psy[1][:, :], r_psy[1])])
                epi.flush()
                P.barrier()


    layer_stack.close()

ATTN_SCALE = 192.0 ** -0.5
NKV = BSTR
NKC = NKV // 128


def phase_mla(P, Dm, C, layer, src, dst, nxt, bg):
    L = layer
    for b in range(2):
        outer = contextlib.ExitStack()
        with outer:
            def osb(name, shape, dt):
                P.uid += 1
                return outer.enter_context(P.nc.sbuf_tensor("%s_%d" % (name, P.uid), list(shape), dt))
            cqn = osb("cqn", [128, 3, SEQ], BF16); r_cqn = Res()
            KnT = osb("KnT", [128, 8, NKV], BF16); r_KnT = Res()
            KpT = osb("KpT", [128, NKV], BF16); r_KpT = Res()
            V = osb("V", [128, NKC, 1024], BF16); r_V = Res()
            with P.phase():
                wd = P.sb("wd", [128, 8, 704], BF16); r_wd = Res()
                for k in range(8):
                    P.dma("pool", wd[:, k, :], Dm["mla_w_dqkv_%d" % L][k * 128:(k + 1) * 128, :], W=[r_wd])
                wsw = P.sb("wsw", [128, 8, 64], BF16)
                P.op("pool", lambda E: E.tensor_copy(out=wsw[:, :, 0:32], in_=wd[:, :, 672:704]), [r_wd], [r_wd])
                P.op("pool", lambda E: E.tensor_copy(out=wsw[:, :, 32:64], in_=wd[:, :, 640:672]), [r_wd], [r_wd])
                wuk = P.sb("wuk", [128, 2, 1024], BF16); wuv = P.sb("wuv", [128, 2, 1024], BF16); r_wu = Res()
                for k in range(2):
                    P.dma("pool", wuk[:, k, :], Dm["mla_w_uk_%d" % L][k * 128:(k + 1) * 128, :], W=[r_wu])
                    P.dma("pool", wuv[:, k, :], Dm["mla_w_uv_%d" % L][k * 128:(k + 1) * 128, :], W=[r_wu])
                bg.attach(3)
                gv = P.sb("gv", [128, 5]); r_gv = Res()
                P.dma("sp", gv[:], Dm["mla_gT_%d" % L], W=[r_gv])
                ones_q = P.sb("ones_q", [128, 128]); ones_kv = P.sb("ones_kv", [128, 128]); r_on = Res()
                P.op("pool", lambda E: E.memset(ones_q[:], 1.0 / 384.0), W=[r_on])
                P.op("pool", lambda E: E.memset(ones_kv[:], 1.0 / 256.0), W=[r_on])
                P.op("pool", lambda E: E.memset(KpT[64:65, :], 1.0), W=[r_KpT])
                hT = [P.sb("hT", [128, 8, 512], BF16) for _ in range(2)]; r_hT = [Res(), Res()]
                dsb = P.sb("dsb", [128, 6, 512]); r_dsb = Res()
                dsw = P.sb("dsw", [128, 512]); r_dsw = Res()
                sq = [P.sb("sq", [128, 512]) for _ in range(2)]; r_sq = [Res(), Res()]
                rq = P.sb("rq", [128, 512]); rkv = P.sb("rkv", [128, 512]); r_rr = Res()
                tmp = [P.sb("tmp", [128, 512]) for _ in range(2)]; r_tmp = [Res(), Res()]
                ckvn = P.sb("ckvn", [128, 2, 512], BF16); r_ckvn = Res()
                rope = P.sb("rope", [64, 2, 512]); r_rope = Res()
                psd = [P.ps("psd", [128, 512]) for _ in range(2)]; r_psd = [Res(), Res()]
                pss = [P.ps("pss", [128, 512]) for _ in range(2)]; r_pss = [Res(), Res()]
                psk = [P.ps("psk", [128, 512]) for _ in range(2)]; r_psk = [Res(), Res()]
                n = 0
                m = 0
                for t0 in range(0, NKV, 512):
                    N = min(512, NKV - t0)
                    isx = t0 < SEQ
                    hb = n % 2
                    n += 1
                    P.dma("sp", hT[hb][:, :, 0:N], Dm["hA"][:, :, b * BSTR + t0:b * BSTR + t0 + N], W=[r_hT[hb]])
                    if isx:
                        P.dma("sp", rope[:], Dm["ropeCS"][:, :, t0:t0 + 512], W=[r_rope])
                    for oc in range(7):
                        bg.step(2)
                        pb = m % 2
                        m += 1
                        M = 128 if oc < 5 else 64
                        for k in range(8):
                            lw = wd[:, k, oc * 128:oc * 128 + M] if oc < 6 else wsw[:, k, :]
                            P.mm(psd[pb][0:M, 0:N], lw, hT[hb][:, k, 0:N], k == 0, k == 7, [r_wd, r_hT[hb]], [r_psd[pb]])
                        if oc < 6:
                            P.op("act", lambda E, pb=pb, M=M, N=N, oc=oc: E.copy(out=dsb[0:M, oc, 0:N], in_=psd[pb][0:M, 0:N]),
                                 [r_psd[pb]], [r_dsb])
                        else:
                            P.op("act", lambda E, pb=pb, N=N: E.copy(out=dsw[0:64, 0:N], in_=psd[pb][0:64, 0:N]),
                                 [r_psd[pb]], [r_dsw])
                        if oc < 5:
                            grp = 0 if oc < 3 else 1
                            first = oc in (0, 3)
                            last = oc in (2, 4)
                            sb_ = oc % 2
                            P.actv(sq[sb_][:, 0:N], dsb[:, oc, 0:N], AF.Square, [r_dsb], [r_sq[sb_]])
                            P.mm(pss[grp][:, 0:N], (ones_q if grp == 0 else ones_kv)[:], sq[sb_][:, 0:N], first, last,
                                 [r_on, r_sq[sb_]], [r_pss[grp]], sig=True)
                    for grp, rr in ((0, rq), (1, rkv)):
                        P.actv(rr[:, 0:N], pss[grp][:, 0:N], AF.Sqrt, [r_pss[grp]], [r_rr], bias=C.eps_ln[:, 2:3], scale=1.0)
                        P.op("dve", lambda E, rr=rr, N=N: E.reciprocal(out=rr[:, 0:N], in_=rr[:, 0:N]), [r_rr], [r_rr])
                    for oc in range(5):
                        tb = oc % 2
                        rr = rq if oc < 3 else rkv
                        if oc < 3 and not isx:
                            continue
                        P.tt("dve", tmp[tb][:, 0:N], dsb[:, oc, 0:N], rr[:, 0:N], ALU.mult, [r_dsb, r_rr], [r_tmp[tb]])
                        if oc < 3:
                            P.actv(cqn[:, oc, t0:t0 + N], tmp[tb][:, 0:N], AF.Identity, [r_tmp[tb], r_gv], [r_cqn],
                                   scale=gv[:, oc:oc + 1])
                        else:
                            P.actv(ckvn[:, oc - 3, 0:N], tmp[tb][:, 0:N], AF.Identity, [r_tmp[tb], r_gv], [r_ckvn],
                                   scale=gv[:, oc:oc + 1])
                    if isx:
                        P.tt("dve", tmp[0][0:64, 0:N], dsb[0:64, 5, 0:N], rope[:, 0, 0:N], ALU.mult, [r_dsb, r_rope], [r_tmp[0]])
                        P.tt("pool", tmp[1][0:64, 0:N], dsw[0:64, 0:N], rope[:, 1, 0:N], ALU.mult, [r_dsw, r_rope], [r_tmp[1]])
                        P.tt("dve", KpT[0:64, t0:t0 + N], tmp[0][0:64, 0:N], tmp[1][0:64, 0:N], ALU.add,
                             [r_tmp[0], r_tmp[1]], [r_KpT])
                    else:
                        P.op("dve", lambda E, N=N, t0=t0: E.tensor_copy(out=KpT[0:64, t0:t0 + N], in_=dsb[0:64, 5, 0:N]),
                             [r_dsb], [r_KpT])
                    for h in range(8):
                        bg.step(2)
                        pb = m % 2
                        m += 1
                        for k in range(2):
                            P.mm(psk[pb][:, 0:N], wuk[:, k, h * 128:(h + 1) * 128], ckvn[:, k, 0:N], k == 0, k == 1,
                                 [r_wu, r_ckvn], [r_psk[pb]])
                        if h % 2 == 0:
                            P.op("act", lambda E, pb=pb, h=h, N=N, t0=t0: E.copy(out=KnT[:, h, t0:t0 + N], in_=psk[pb][:, 0:N]),
                                 [r_psk[pb]], [r_KnT])
                        else:
                            P.op("dve", lambda E, pb=pb, h=h, N=N, t0=t0: E.tensor_copy(out=KnT[:, h, t0:t0 + N], in_=psk[pb][:, 0:N]),
                                 [r_psk[pb]], [r_KnT])
                    for jt in range(N // 128):
                        for hf in range(2):
                            pb = m % 2
                            m += 1
                            for k in range(2):
                                P.mm(psk[pb][:, :], ckvn[:, k, jt * 128:(jt + 1) * 128], wuv[:, k, hf * 512:(hf + 1) * 512],
                                     k == 0, k == 1, [r_ckvn, r_wu], [r_psk[pb]])
                            kc = t0 // 128 + jt
                            if hf == 0:
                                P.op("act", lambda E, pb=pb, kc=kc: E.copy(out=V[:, kc, 0:512], in_=psk[pb][:, :]),
                                     [r_psk[pb]], [r_V])
                            else:
                                P.op("dve", lambda E, pb=pb, kc=kc: E.tensor_copy(out=V[:, kc, 512:1024], in_=psk[pb][:, :]),
                                     [r_psk[pb]], [r_V])
                bg.detach()
                P.barrier()
            with P.phase():
                wuq = P.sb("wuq", [128, 3, 1536], BF16); r_wq = Res()
                for k in range(3):
                    P.dma("pool", wuq[:, k, :], Dm["mla_w_uq_%d" % L][k * 128:(k + 1) * 128, :], W=[r_wq])
                wqsw = P.sb("wqsw", [128, 3, 512], BF16)
                for h in range(8):
                    P.op("pool", lambda E, h=h: E.tensor_copy(out=wqsw[:, :, h * 64:h * 64 + 32],
                                                             in_=wuq[:, :, h * 192 + 160:h * 192 + 192]), [r_wq], [r_wq])
                    P.op("pool", lambda E, h=h: E.tensor_copy(out=wqsw[:, :, h * 64 + 32:h * 64 + 64],
                                                             in_=wuq[:, :, h * 192 + 128:h * 192 + 160]), [r_wq], [r_wq])
                wo = P.sb("wo", [128, 8, 1024], BF16); r_wo = Res()
                for k in range(8):
                    P.dma("pool", wo[:, k, :], Dm["mla_w_o_%d" % L][k * 128:(k + 1) * 128, :], W=[r_wo])
                ones_b = P.sb("ones_b", [128, 128], BF16); r_on = Res()
                P.op("pool", lambda E: E.memset(ones_b[:], 1.0), W=[r_on])
                QnT = P.sb("QnT", [128, 8, 512], BF16); r_Qn = Res()
                QpT = P.sb("QpT", [128, 8, 512], BF16); r_Qp = Res()
                OT = P.sb("OT", [128, 8, 512], BF16); r_OT = Res()
                PT = [P.sb("PT", [128, 512], BF16) for _ in range(3)]; r_PT = [Res() for _ in range(3)]
                rope = P.sb("rope", [64, 2, 512]); r_rope = Res()
                sqb = [P.sb("sqb", [128, 512], BF16) for _ in range(2)]; r_sqb = [Res(), Res()]
                t1 = P.sb("t1", [128, 512]); t2 = P.sb("t2", [128, 512]); r_t = [Res(), Res()]
                rden = P.sb("rden", [128, 512]); r_rden = Res()
                kmx = P.sb("kmx", [128, 8, 8]); r_kmx = Res()
                negK = P.sb("negK", [128, 8]); r_negK = Res()
                pss = [P.ps("pss", [128, 512]) for _ in range(2)]; r_pss = [Res(), Res()]
                pso = P.ps("pso", [128, 512]); r_pso = Res()
                psden = P.ps("psden", [128, 512]); r_psden = Res()
                psy = [P.ps("psy", [128, 512]) for _ in range(2)]; r_psy = [Res(), Res()]
                psT = [P.ps("psT", [128, 512]) for _ in range(2)]; r_psT = [Res(), Res()]
                epi = Epi(P, Dm, C, layer, 1, nxt, True, src, dst, Dm["hB"], psT, r_psT, psden, r_psden, ns=2)
                P.op("pool", lambda E: E.memset(kmx[:], 0.0), W=[r_kmx])
                m = 0
                for h in range(8):
                    for ti, t0 in enumerate(range(0, NKV, 512)):
                        N = min(512, NKV - t0)
                        pb = m % 2
                        m += 1
                        P.actv(sqb[0][:, 0:N], KnT[:, h, t0:t0 + N], AF.Square, [r_KnT], [r_sqb[0]])
                        P.actv(sqb[1][0:64, 0:N], KpT[0:64, t0:t0 + N], AF.Square, [r_KpT], [r_sqb[1]])
                        P.mm(pss[pb][:, 0:N], ones_b[:], sqb[0][:, 0:N], True, False, [r_on, r_sqb[0]], [r_pss[pb]], sig=False)
                        P.mm(pss[pb][:, 0:N], ones_b[0:64, :], sqb[1][0:64, 0:N], False, True, [r_on, r_sqb[1]], [r_pss[pb]])
                        P.op("dve", lambda E, pb=pb, N=N, h=h, ti=ti: E.tensor_reduce(
                            out=kmx[:, h, ti:ti + 1], in_=pss[pb][:, 0:N], axis=AX.X, op=ALU.max), [r_pss[pb]], [r_kmx])
                P.op("dve", lambda E: E.tensor_reduce(out=negK[:], in_=kmx[:], axis=AX.X, op=ALU.max), [r_kmx], [r_negK])
                P.actv(negK[:], negK[:], AF.Sqrt, [r_negK], [r_negK])
                P.ts("dve", negK[:], negK[:], -1.02, None, ALU.mult, None, [r_negK], [r_negK])
                nonlocal_m = [m]
                nonlocal_p = [0]
                for qt in range(4):
                    q0 = qt * 512
                    m = nonlocal_m[0] + 1
                    P.dma("sp", rope[:], Dm["ropeCS"][:, :, q0:q0 + 512], W=[r_rope])
                    for h in range(8):
                        pb = m % 2
                        m += 1
                        for k in range(3):
                            P.mm(pss[pb][:, :], wuq[:, k, h * 192:h * 192 + 128], cqn[:, k, q0:q0 + 512], k == 0, k == 2,
                                 [r_wq, r_cqn], [r_pss[pb]])
                        P.op("act", lambda E, pb=pb, h=h: E.copy(out=QnT[:, h, :], in_=pss[pb][:, :]), [r_pss[pb]], [r_Qn])
                        pb2 = m % 2
                        m += 1
                        for k in range(3):
                            P.mm(pss[pb2][0:64, :], wuq[:, k, h * 192 + 128:h * 192 + 192], cqn[:, k, q0:q0 + 512],
                                 k == 0, k == 2, [r_wq, r_cqn], [r_pss[pb2]])
                        P.tt("dve", t1[0:64, :], pss[pb2][0:64, :], rope[:, 0, :], ALU.mult, [r_pss[pb2], r_rope], [r_t[0]])
                        pb3 = m % 2
                        m += 1
                        for k in range(3):
                            P.mm(pss[pb3][0:64, :], wqsw[:, k, h * 64:(h + 1) * 64], cqn[:, k, q0:q0 + 512],
                                 k == 0, k == 2, [r_wq, r_cqn], [r_pss[pb3]])
                        P.tt("dve", t2[0:64, :], pss[pb3][0:64, :], rope[:, 1, :], ALU.mult, [r_pss[pb3], r_rope], [r_t[1]])
                        P.tt("pool", QpT[0:64, h, :], t1[0:64, :], t2[0:64, :], ALU.add, [r_t[0], r_t[1]], [r_Qp])
                        P.actv(sqb[0][:, :], QnT[:, h, :], AF.Square, [r_Qn], [r_sqb[0]])
                        P.actv(sqb[1][0:64, :], QpT[0:64, h, :], AF.Square, [r_Qp], [r_sqb[1]])
                        pb4 = m % 2
                        m += 1
                        P.mm(pss[pb4][:, :], ones_b[:], sqb[0][:, :], True, False, [r_on, r_sqb[0]], [r_pss[pb4]], sig=False)
                        P.mm(pss[pb4][:, :], ones_b[0:64, :], sqb[1][0:64, :], False, True, [r_on, r_sqb[1]], [r_pss[pb4]])
                        P.actv(t1[64:65, :], pss[pb4][64:65, :], AF.Sqrt, [r_pss[pb4]], [r_t[0]])
                        P.ts("dve", QpT[64:65, h, :], t1[64:65, :], negK[64:65, h:h + 1], None, ALU.mult, None,
                             [r_t[0], r_negK], [r_Qp])
                    nonlocal_m[0] = m
                    for h in range(8):
                        def emit_S(kc, h=h):
                            nonlocal_m[0] += 1
                            pb = nonlocal_m[0] % 2
                            P.mm(pss[pb][:, :], KnT[:, h, kc * 128:(kc + 1) * 128], QnT[:, h, :], True, False,
                                 [r_KnT, r_Qn], [r_pss[pb]], sig=False)
                            P.mm(pss[pb][:, :], KpT[0:65, kc * 128:(kc + 1) * 128], QpT[0:65, h, :], False, True,
                                 [r_KpT, r_Qp], [r_pss[pb]])
                            pi = nonlocal_p[0] % 3
                            nonlocal_p[0] += 1
                            P.actv(PT[pi][:], pss[pb][:, :], AF.Exp, [r_pss[pb]], [r_PT[pi]], scale=ATTN_SCALE)
                            return pi
                        pis = {0: emit_S(0)}
                        for kc in range(NKC):
                            if kc + 1 < NKC:
                                pis[kc + 1] = emit_S(kc + 1)
                            pi = pis[kc]
                            P.mm(pso[:, :], V[:, kc, h * 128:(h + 1) * 128], PT[pi][:], kc == 0, kc == NKC - 1,
                                 [r_V, r_PT[pi]], [r_pso], sig=True)
                            P.mm(psden[:, :], ones_b[:], PT[pi][:], kc == 0, kc == NKC - 1,
                                 [r_on, r_PT[pi]], [r_psden], sig=True)
                        P.op("dve", lambda E: E.reciprocal(out=rden[:], in_=psden[:, :]), [r_psden], [r_rden])
                        P.tt("dve", OT[:, h, :], pso[:, :], rden[:], ALU.mult, [r_pso, r_rden], [r_OT])
                    for jt in range(4):
                        for hf in range(2):
                            for k in range(8):
                                P.mm(psy[hf][:, :], OT[:, k, jt * 128:(jt + 1) * 128], wo[:, k, hf * 512:(hf + 1) * 512],
                                     k == 0, k == 7, [r_OT, r_wo], [r_psy[hf]])
                        epi.tile(b * BSTR + q0 + jt * 128, [(psy[0][:, :], r_psy[0]), (psy[1][:, :], r_psy[1])])
                epi.flush()
                P.barrier()

def moe_supertiles(layer):
    runs = active_runs(layer)
    tiles = []
    for (s0, ln) in runs:
        tiles += list(range(s0, s0 + ln, 128))
    sts = []
    for i in range(0, len(tiles), 12):
        sts.append(tiles[i:i + 12])
    return sts


def phase_moe(P, Dm, C, layer, src, dst, nxt):
    for stiles in moe_supertiles(layer):
        nt = len(stiles)
        ngrp = nt // 4
        with P.phase():
            hT = P.sb("hT", [128, 8, nt * 128], BF16); r_hT = Res()
            gates = P.sb("gates", [128, nt, 64]); r_g = Res()
            i0 = 0
            while i0 < nt:
                i1 = i0
                while i1 + 1 < nt and stiles[i1 + 1] == stiles[i1] + 128:
                    i1 += 1
                ta, tb = stiles[i0], stiles[i1] + 128
                for k in range(8):
                    P.dma("sp" if k % 2 == 0 else "act", hT[:, k, i0 * 128:(i1 + 1) * 128], Dm["hB"][:, k, ta:tb],
                          W=[r_hT])
                P.dma("sp", gates[:, i0:i1 + 1, :], Dm["gates"][ta:tb, :].rearrange("(n p) e -> p n e", p=128), W=[r_g])
                i0 = i1 + 1
            yacc = P.sb("yacc", [128, nt, 1024]); r_y = [Res() for _ in range(nt)]
            NWB = 3
            w1 = [P.sb("w1", [128, 8, 256], BF16) for _ in range(NWB)]
            w3 = [P.sb("w3", [128, 8, 256], BF16) for _ in range(NWB)]
            w2 = [P.sb("w2", [128, 2, 1024], BF16) for _ in range(NWB)]
            r_w13 = [Res() for _ in range(NWB)]
            r_w2 = [Res() for _ in range(NWB)]
            gT = [P.sb("gT", [128, 2, 512], BF16) for _ in range(2)]; r_gT = [[Res(), Res()] for _ in range(2)]
            sg = [P.sb("sg", [128, 512]) for _ in range(2)]; r_sg = [Res(), Res()]
            psh = [[P.ps("psh", [128, 512]) for _ in range(2)] for _ in range(2)]
            r_psh = [[Res(), Res()] for _ in range(2)]
            psy = [P.ps("psy", [128, 512]) for _ in range(4)]; r_psy = [Res() for _ in range(4)]
            ytmp = [P.sb("ytmp", [128, 512]) for _ in range(4)]; r_ytmp = [Res() for _ in range(4)]
            POOL_SLOTS = (1, 4, 6)

            def load_w(e, wb):
                if e < 0:
                    a1, a3, a2 = Dm["sh_w1_%d" % layer], Dm["sh_w3_%d" % layer], Dm["sh_w2_%d" % layer]
                else:
                    a1, a3, a2 = Dm["moe_w1_%d" % layer][e], Dm["moe_w3_%d" % layer][e], Dm["moe_w2_%d" % layer][e]
                P.dma("pool", w1[wb][:], a1.rearrange("(k p) f -> p k f", p=128), W=[r_w13[wb]])
                P.dma("pool", w3[wb][:], a3.rearrange("(k p) f -> p k f", p=128), W=[r_w13[wb]])
                P.dma("pool", w2[wb][:], a2.rearrange("(k p) f -> p k f", p=128), W=[r_w2[wb]])

            units = [(e, g) for e in range(-1, 64) for g in range(ngrp)]
            ycnt = [0]

            def emit_H(u, ui):
                e, g = u
                wb = (e + 1) % NWB
                gb = ui % 2
                for c in range(2):
                    for (wt, pi) in ((w1, 0), (w3, 1)):
                        for k in range(8):
                            P.mm(psh[c][pi][:, :], wt[wb][:, k, c * 128:(c + 1) * 128], hT[:, k, g * 512:(g + 1) * 512],
                                 k == 0, k == 7, [r_w13[wb], r_hT], [r_psh[c][pi]])
                    P.actv(sg[c][:], psh[c][0][:, :], AF.Silu, [r_psh[c][0]], [r_sg[c]])
                    P.tt("dve", gT[gb][:, c, :], psh[c][1][:, :], sg[c][:], ALU.mult, [r_psh[c][1], r_sg[c]],
                         [r_gT[gb][c]])

            def emit_Y(u, ui):
                e, g = u
                wb = (e + 1) % NWB
                gb = ui % 2
                for jt in range(4):
                    ti = g * 4 + jt
                    for hf in range(2):
                        yb = ycnt[0] % 4
                        ycnt[0] += 1
                        for c in range(2):
                            P.mm(psy[yb][:, :], gT[gb][:, c, jt * 128:(jt + 1) * 128],
                                 w2[wb][:, c, hf * 512:(hf + 1) * 512], c == 0, c == 1,
                                 [r_gT[gb][c], r_w2[wb]], [r_psy[yb]])
                        ydst = yacc[:, ti, hf * 512:(hf + 1) * 512]
                        if e < 0:
                            P.op("act", lambda E, ydst=ydst, yb=yb: E.copy(out=ydst, in_=psy[yb][:, :]),
                                 [r_psy[yb]], [r_y[ti]])
                        else:
                            P.actv(ytmp[yb][:], psy[yb][:, :], AF.Copy, [r_psy[yb], r_g], [r_ytmp[yb]],
                                   scale=gates[:, ti, e:e + 1])
                            aeng = "pool" if (jt * 2 + hf) in POOL_SLOTS else "dve"
                            P.tt(aeng, ydst, ydst, ytmp[yb][:], ALU.add, [r_y[ti], r_ytmp[yb]], [r_y[ti]])

            loaded = set()

            def need_w(e):
                if e not in loaded and e < 64:
                    loaded.add(e)
                    load_w(e, (e + 1) % NWB)
            need_w(-1)
            need_w(0)
            for ui, u in enumerate(units):
                if ui == 0:
                    emit_H(u, ui)
                if ui + 1 < len(units):
                    nu = units[ui + 1]
                    need_w(nu[0])
                    emit_H(nu, ui + 1)
                emit_Y(u, ui)
                if u[1] == ngrp - 1:
                    need_w(u[0] + 2)
            epi = Epi(P, Dm, C, layer, 2, nxt, False, src, dst, Dm["hA"], [psh[0][0], psh[0][1]],
                      [r_psh[0][0], r_psh[0][1]])
            for ti, t0 in enumerate(stiles):
                epi.tile(t0, [(yacc[:, ti, 0:512], r_y[ti]), (yacc[:, ti, 512:1024], r_y[ti])])
            epi.flush()
            P.barrier()


IOA = bass.IndirectOffsetOnAxis
I32 = mybir.dt.int32
WROW = 6144


class BgW:
    def __init__(self, P, Dm, layer):
        self.P, self.Dm, self.layer = P, Dm, layer
        self.steps = [(e, part) for e in range(64) for part in range(3)]
        self.pos = 0
        self.pending = None
        self.bufs = None
        self.k = 0

    def attach(self, nbuf=3):
        self.bufs = [self.P.sb("bgw", [128, 2048], BF16) for _ in range(nbuf)]
        self.res = [Res() for _ in range(nbuf)]

    def _flush(self):
        if self.pending is not None:
            e, part, b = self.pending
            self.P.dma("sp", self.Dm["WS%d" % (self.layer % 2)][e * 128:(e + 1) * 128, part * 2048:(part + 1) * 2048],
                       self.bufs[b][:], R=[self.res[b]])
            self.pending = None

    def step(self, n=1):
        for _ in range(n):
            self._flush()
            if self.pos >= len(self.steps):
                return
            e, part = self.steps[self.pos]
            self.pos += 1
            b = self.k % len(self.bufs)
            self.k += 1
            L = self.layer
            if part == 0:
                src, kk = self.Dm["moe_w1_%d" % L][e], 8
            elif part == 1:
                src, kk = self.Dm["moe_w3_%d" % L][e], 8
            else:
                src, kk = self.Dm["moe_w2_%d" % L][e], 2
            self.P.dma("pool", self.bufs[b][:].rearrange("p (k f) -> p k f", k=kk),
                       src.rearrange("(k p) f -> p k f", p=128), W=[self.res[b]])
            self.pending = (e, part, b)

    def detach(self):
        self._flush()
        self.bufs = None

    def done(self):
        return self.pos >= len(self.steps) and self.pending is None


def moe_tiles(layer):
    return [t for (s0, ln) in active_runs(layer) for t in range(s0, s0 + ln, 128)]


def phase_wtable(P, Dm, C, layer, bg):
    if bg.done():
        return
    with P.phase():
        bg.attach(6)
        while not bg.done():
            bg.step(1)
        bg.detach()
        P.barrier()


def phase_dispatch(P, Dm, C, layer):
    tl = moe_tiles(layer)
    NT = len(tl)
    NB = NT * 8 + 64
    runs = active_runs(layer)
    with P.phase():
        r_c = Res()
        Ustr = P.sb("Ustr", [128, 128], BF16); onesb = P.sb("onesb", [128, 128], BF16)
        U64 = P.sb("U64", [64, 64]); UI64 = P.sb("UI64", [64, 64]); ones64 = P.sb("ones64", [64, 128])
        piota = P.sb("piota", [128, 1]); bstart = P.sb("bstart", [64, NB])
        for (t, npart, nfree, cmp_) in ((Ustr, 128, 128, ALU.is_gt), (U64, 64, 64, ALU.is_gt), (UI64, 64, 64, ALU.is_ge)):
            P.op("pool", lambda E, t=t: E.memset(t[:], 1.0), W=[r_c])
            P.op("pool", lambda E, t=t, nfree=nfree, cmp_=cmp_: E.affine_select(
                out=t[:], in_=t[:], pattern=[[1, nfree]], compare_op=cmp_, fill=0.0, base=0, channel_multiplier=-1),
                R=[r_c], W=[r_c])
        P.op("pool", lambda E: E.memset(onesb[:], 1.0), W=[r_c])
        P.op("pool", lambda E: E.memset(ones64[:], 1.0), W=[r_c])
        P.op("pool", lambda E: E.iota(piota[:], pattern=[[0, 1]], base=0, channel_multiplier=1,
                                      allow_small_or_imprecise_dtypes=True), W=[r_c])
        P.op("pool", lambda E: E.iota(bstart[:], pattern=[[128, NB]], base=0, channel_multiplier=0,
                                      allow_small_or_imprecise_dtypes=True), W=[r_c])
        Gall = P.sb("Gall", [128, NT, 64]); r_G = Res()
        Mall = P.sb("Mall", [128, NT, 64], BF16); r_M = Res()
        Sall = P.sb("Sall", [128, NT, 64], BF16); r_S = Res()
        key = P.sb("key", [128, NT, 64]); r_key = [Res() for _ in range(NT)]
        d8k = P.sb("d8k", [128, NT, 8]); r_d8k = Res()
        g8 = P.sb("g8", [128, NT, 8]); r_g8 = Res()
        d8i = P.sb("d8i", [128, NT, 8], I32); r_d8i = Res()
        junk = [P.sb("junk", [128, 64]) for _ in range(2)]; r_junk = [Res(), Res()]
        pcnt = P.ps("pcnt", [128, 512]); r_pcnt = Res()
        ppos = [P.ps("ppos", [128, 512]) for _ in range(2)]; r_ppos = [Res(), Res()]
        pmisc = P.ps("pmisc", [128, 512]); r_pmisc = Res()
        n0 = 0
        for (s0, ln) in runs:
            k = ln // 128
            P.dma("sp", Gall[:, n0:n0 + k, :], Dm["gates"][s0:s0 + ln, :].rearrange("(n p) e -> p n e", p=128), W=[r_G])
            n0 += k
        fl = lambda t: t[:].rearrange("p n e -> p (n e)")
        P.op("dve", lambda E: E.tensor_single_scalar(out=fl(Mall), in_=fl(Gall), scalar=0.0, op=ALU.is_gt), [r_G], [r_M])
        P.op("pool", lambda E: E.memset(Sall[:, 0, :], 0.0), W=[r_S])
        P.op("pool", lambda E: E.memset(fl(g8), 0.0), W=[r_g8])
        for n in range(1, NT):
            P.tt("dve", Sall[:, n, :], Sall[:, n - 1, :], Mall[:, n - 1, :], ALU.add, [r_S, r_M], [r_S])
        for n in range(NT):
            P.mm(pcnt[0:64, 0:2], Mall[:, n, :], onesb[:, 0:2], n == 0, n == NT - 1, [r_M, r_c], [r_pcnt])
        cnt = P.sb("cnt", [64, 2]); rr = P.sb("rr", [64, 2]); pad = P.sb("pad", [64, 2]); r_pad = Res()
        padB = P.sb("padB", [64, 128]); pend = P.sb("pend", [64, 2]); ps1 = P.sb("ps1", [128, 64]); r_ps1 = Res()
        MAGIC = 12582912.0
        P.ts("dve", cnt[:], pcnt[0:64, 0:2], 1.0 / 128.0, 127.0 / 256.0, ALU.mult, ALU.add, [r_pcnt], [r_pad])
        P.ts("dve", rr[:], cnt[:], MAGIC, None, ALU.add, None, [r_pad], [r_pad])
        P.ts("dve", pad[:], rr[:], -MAGIC, 128.0, ALU.add, ALU.mult, [r_pad], [r_pad])
        P.ts("dve", padB[:], ones64[:], pad[:, 0:1], None, ALU.mult, None, [r_c, r_pad], [r_pad])
        P.mm(pmisc[:, 0:64], padB[:], U64[:], True, True, [r_pad, r_c], [r_pmisc])
        P.ts("dve", ps1[:], pmisc[:, 0:64], 1.0, None, ALU.add, None, [r_pmisc], [r_ps1])
        P.mm(pmisc[0:64, 64:66], UI64[:], pad[:], True, True, [r_pad, r_c], [r_pmisc])
        P.op("dve", lambda E: E.tensor_copy(out=pend[:], in_=pmisc[0:64, 64:66]), [r_pmisc], [r_pad])
        cmpT = P.sb("cmpT", [64, NB], BF16); idxf = P.sb("idxf", [128, NB]); idxw = P.sb("idxw", [128, NB], I32); r_ix = Res()
        P.ts("dve", cmpT[:], bstart[:], pend[:, 0:1], None, ALU.is_ge, None, [r_c, r_pad], [r_ix])
        P.mm(pmisc[:, 0:NB], onesb[0:64, :], cmpT[:], True, True, [r_c, r_ix], [r_pmisc])
        P.ts("dve", idxf[:], pmisc[:, 0:NB], 63.0, 128.0, ALU.min, ALU.mult, [r_pmisc], [r_ix])
        P.ts("dve", idxw[:], idxf[:], piota[:, 0:1], None, ALU.add, None, [r_ix, r_c], [r_ix])
        P.dma("sp", Dm["idxw"][:, 0:NB], idxw[:], R=[r_ix])
        for n in range(NT):
            pb = n % 2
            P.mm(ppos[pb][:, 0:64], Ustr[:], Mall[:, n, :], True, False, [r_c, r_M], [r_ppos[pb]], sig=False)
            P.mm(ppos[pb][:, 0:64], onesb[:], Sall[:, n, :], False, True, [r_c, r_S], [r_ppos[pb]])
            P.tt("dve", key[:, n, :], ppos[pb][:, 0:64], ps1[:], ALU.add, [r_ppos[pb], r_ps1], [r_key[n]])
            P.tt("dve", key[:, n, :], key[:, n, :], Mall[:, n, :], ALU.mult, [r_key[n], r_M], [r_key[n]])
            P.op("dve", lambda E, n=n: E.max(out=d8k[:, n, :], in_=key[:, n, :]), [r_key[n]], [r_d8k])
            for j in range(8):
                jb = j % 2
                P.op("dve", lambda E, n=n, j=j, jb=jb: E.scalar_tensor_tensor(
                    out=junk[jb][:], in0=key[:, n, :], scalar=d8k[:, n, j:j + 1], in1=Gall[:, n, :],
                    op0=ALU.is_equal, op1=ALU.mult, accum_out=g8[:, n, j:j + 1]),
                    [r_key[n], r_d8k, r_G], [r_junk[jb], r_g8])
        P.ts("dve", d8i[:].rearrange("p n j -> p (n j)"), d8k[:].rearrange("p n j -> p (n j)"), -1.0, None, ALU.add, None,
             [r_d8k], [r_d8i])
        n0 = 0
        for (s0, ln) in runs:
            k = ln // 128
            P.dma("sp", Dm["d8i"][s0:s0 + ln, :].rearrange("(n p) j -> p n j", p=128), d8i[:, n0:n0 + k, :], R=[r_d8i])
            P.dma("sp", Dm["g8"][s0:s0 + ln, :].rearrange("(n p) j -> p n j", p=128), g8[:, n0:n0 + k, :], R=[r_g8])
            n0 += k
        hrt = [P.sb("hrt", [128, 1024], BF16) for _ in range(3)]; r_hrt = [Res() for _ in range(3)]
        r_hbuf = Res()
        for n, t0 in enumerate(tl):
            b = n % 3
            P.dma("sp", hrt[b][:], Dm["hrow"][t0:t0 + 128, :], W=[r_hrt[b]])
            for j in range(8):
                P.idma(Dm["hbuf"], IOA(d8i[:, n, j:j + 1], 0), hrt[b][:], None, R=[r_hrt[b], r_d8i], W=[])
        P.barrier()
    return NB


def phase_blocks(P, Dm, C, layer, NB):
    with P.phase():
        idxw = P.sb("idxw", [128, NB], I32); r_ix = Res()
        P.dma("sp", idxw[:], Dm["idxw"][:, 0:NB], W=[r_ix])
        NWB = 6
        NHB = 6
        wb = [P.sb("wb", [128, WROW], BF16) for _ in range(NWB)]; r_wb = [Res() for _ in range(NWB)]
        hblk = [P.sb("hblk", [128, 1024], BF16) for _ in range(NHB)]; r_hblk = [Res() for _ in range(NHB)]
        hTk = [P.sb("hTk", [128, 8, 128], BF16) for _ in range(2)]; r_hTk = [Res(), Res()]
        sg = [P.sb("sg", [128, 256]) for _ in range(2)]; r_sg = [Res(), Res()]
        gTb = [P.sb("gTb", [128, 256], BF16) for _ in range(2)]; r_gTb = [Res(), Res()]
        ysb = [P.sb("ysb", [128, 1024]) for _ in range(4)]; r_ysb = [Res() for _ in range(4)]
        psT = [P.ps("psTb", [128, 1024], BF16) for _ in range(2)]; r_psT = [Res(), Res()]
        ph = [P.ps("ph", [128, 512]) for _ in range(2)]; r_ph = [Res(), Res()]
        psy = [P.ps("psy", [128, 512]) for _ in range(4)]; r_psy = [Res() for _ in range(4)]

        def load(b):
            P.idma(wb[b % NWB][:], None, Dm["WS%d" % (layer % 2)], IOA(idxw[:, b:b + 1], 0), R=[r_ix], W=[r_wb[b % NWB]])
            P.dma("sp", hblk[b % NHB][:], Dm["hbuf"][b * 128:(b + 1) * 128, :], W=[r_hblk[b % NHB]])

        def emit_TH(b):
            i2 = b % 2
            w = wb[b % NWB]
            hb = hblk[b % NHB]
            for k in range(8):
                P.op("pe", lambda E, k=k: E.transpose(out=psT[i2][:, k * 128:(k + 1) * 128], in_=hb[:, k * 128:(k + 1) * 128],
                                                      identity=C.identb[:]),
                     [r_hblk[b % NHB], C.r_const], [r_psT[i2]], sig=(k == 7))
            P.op("act", lambda E: E.copy(out=hTk[i2][:, 0:4, :], in_=psT[i2][:, 0:512].rearrange("p (k t) -> p k t", k=4)),
                 [r_psT[i2]], [r_hTk[i2]])
            P.op("dve", lambda E: E.tensor_copy(out=hTk[i2][:, 4:8, :], in_=psT[i2][:, 512:1024].rearrange("p (k t) -> p k t", k=4)),
                 [r_psT[i2]], [r_hTk[i2]])
            for wi in range(2):
                for c in range(2):
                    for k in range(8):
                        col = wi * 2048 + k * 256 + c * 128
                        P.mm(ph[i2][:, wi * 256 + c * 128:wi * 256 + (c + 1) * 128], w[:, col:col + 128], hTk[i2][:, k, :],
                             k == 0, k == 7, [r_wb[b % NWB], r_hTk[i2]], [r_ph[i2]], sig=(k == 7 and c == 1))
            P.actv(sg[i2][:], ph[i2][:, 0:256], AF.Silu, [r_ph[i2]], [r_sg[i2]])
            P.tt("dve", gTb[i2][:], ph[i2][:, 256:512], sg[i2][:], ALU.mult, [r_ph[i2], r_sg[i2]], [r_gTb[i2]])

        def emit_Y(b):
            i2 = b % 2
            w = wb[b % NWB]
            for hf in range(2):
                yb = (b % 2) * 2 + hf
                for c in range(2):
                    col = 4096 + c * 1024 + hf * 512
                    P.mm(psy[yb][:, :], gTb[i2][:, c * 128:(c + 1) * 128], w[:, col:col + 512], c == 0, c == 1,
                         [r_gTb[i2], r_wb[b % NWB]], [r_psy[yb]])
                i4 = b % 4
                if hf == 0:
                    P.op("act", lambda E, yb=yb, i4=i4: E.copy(out=ysb[i4][:, 0:512], in_=psy[yb][:, :]), [r_psy[yb]], [r_ysb[i4]])
                else:
                    P.op("dve", lambda E, yb=yb, i4=i4: E.tensor_copy(out=ysb[i4][:, 512:1024], in_=psy[yb][:, :]),
                         [r_psy[yb]], [r_ysb[i4]])
            P.dma("act", Dm["ybuf"][b * 128:(b + 1) * 128, :], ysb[b % 4][:], R=[r_ysb[b % 4]])

        PF = 5
        for b in range(PF):
            load(b)
        emit_TH(0)
        for b in range(NB):
            if b + PF < NB:
                load(b + PF)
            if b + 1 < NB:
                emit_TH(b + 1)
            emit_Y(b)
        P.barrier()


def phase_combine(P, Dm, C, layer, src, dst, nxt):
    for stiles in moe_supertiles(layer):
        nt = len(stiles)
        ngrp = nt // 4
        with P.phase():
            hT = P.sb("hT", [128, 8, nt * 128], BF16); r_hT = Res()
            d8i = P.sb("d8i", [128, nt, 8], I32); g8 = P.sb("g8", [128, nt, 8]); r_g = Res()
            i0 = 0
            while i0 < nt:
                i1 = i0
                while i1 + 1 < nt and stiles[i1 + 1] == stiles[i1] + 128:
                    i1 += 1
                ta, tb = stiles[i0], stiles[i1] + 128
                for k in range(8):
                    P.dma("sp" if k % 2 == 0 else "act", hT[:, k, i0 * 128:(i1 + 1) * 128], Dm["hB"][:, k, ta:tb], W=[r_hT])
                P.dma("sp", d8i[:, i0:i1 + 1, :], Dm["d8i"][ta:tb, :].rearrange("(n p) j -> p n j", p=128), W=[r_g])
                P.dma("sp", g8[:, i0:i1 + 1, :], Dm["g8"][ta:tb, :].rearrange("(n p) j -> p n j", p=128), W=[r_g])
                i0 = i1 + 1
            yacc = P.sb("yacc", [128, nt, 1024]); r_y = [Res() for _ in range(nt)]
            w1 = P.sb("w1", [128, 8, 256], BF16); w3 = P.sb("w3", [128, 8, 256], BF16); w2 = P.sb("w2", [128, 2, 1024], BF16)
            r_w = Res()
            P.dma("pool", w1[:], Dm["sh_w1_%d" % layer].rearrange("(k p) f -> p k f", p=128), W=[r_w])
            P.dma("pool", w3[:], Dm["sh_w3_%d" % layer].rearrange("(k p) f -> p k f", p=128), W=[r_w])
            P.dma("pool", w2[:], Dm["sh_w2_%d" % layer].rearrange("(k p) f -> p k f", p=128), W=[r_w])
            gT = [P.sb("gT", [128, 2, 512], BF16) for _ in range(2)]; r_gT = [Res(), Res()]
            sg = [P.sb("sg", [128, 512]) for _ in range(2)]; r_sg = [Res(), Res()]
            NYG = 8
            yg = [P.sb("yg", [128, 1024]) for _ in range(NYG)]; r_yg = [Res() for _ in range(NYG)]
            psh = [[P.ps("psh", [128, 512]) for _ in range(2)] for _ in range(2)]
            r_psh = [[Res(), Res()] for _ in range(2)]
            psy = [P.ps("psy", [128, 512]) for _ in range(4)]; r_psy = [Res() for _ in range(4)]
            ycnt = 0
            for g in range(ngrp):
                gb = g % 2
                for c in range(2):
                    for (wt, pi) in ((w1, 0), (w3, 1)):
                        for k in range(8):
                            P.mm(psh[c][pi][:, :], wt[:, k, c * 128:(c + 1) * 128], hT[:, k, g * 512:(g + 1) * 512],
                                 k == 0, k == 7, [r_w, r_hT], [r_psh[c][pi]])
                    P.actv(sg[c][:], psh[c][0][:, :], AF.Silu, [r_psh[c][0]], [r_sg[c]])
                    P.tt("dve", gT[gb][:, c, :], psh[c][1][:, :], sg[c][:], ALU.mult, [r_psh[c][1], r_sg[c]], [r_gT[gb]])
                for jt in range(4):
                    ti = g * 4 + jt
                    for hf in range(2):
                        yb = ycnt % 4
                        ycnt += 1
                        for c in range(2):
                            P.mm(psy[yb][:, :], gT[gb][:, c, jt * 128:(jt + 1) * 128], w2[:, c, hf * 512:(hf + 1) * 512],
                                 c == 0, c == 1, [r_gT[gb], r_w], [r_psy[yb]])
                        ydst = yacc[:, ti, hf * 512:(hf + 1) * 512]
                        if hf == 0:
                            P.op("act", lambda E, ydst=ydst, yb=yb: E.copy(out=ydst, in_=psy[yb][:, :]), [r_psy[yb]], [r_y[ti]])
                        else:
                            P.op("dve", lambda E, ydst=ydst, yb=yb: E.tensor_copy(out=ydst, in_=psy[yb][:, :]),
                                 [r_psy[yb]], [r_y[ti]])
            items = [(ti, j) for ti in range(nt) for j in range(8)]
            epi = Epi(P, Dm, C, layer, 2, nxt, False, src, dst, Dm["hA"], [psh[0][0], psh[0][1]],
                      [r_psh[0][0], r_psh[0][1]])

            def gather(ix):
                ti, j = items[ix]
                P.idma(yg[ix % NYG][:], None, Dm["ybuf"], IOA(d8i[:, ti, j:j + 1], 0), R=[r_g], W=[r_yg[ix % NYG]])
            for ix in range(min(NYG - 1, len(items))):
                gather(ix)
            for ix, (ti, j) in enumerate(items):
                if ix + NYG - 1 < len(items):
                    gather(ix + NYG - 1)
                yb = ix % NYG
                P.op("dve", lambda E, ti=ti, j=j, yb=yb: E.scalar_tensor_tensor(
                    out=yacc[:, ti, :], in0=yg[yb][:], scalar=g8[:, ti, j:j + 1], in1=yacc[:, ti, :],
                    op0=ALU.mult, op1=ALU.add), [r_yg[yb], r_g, r_y[ti]], [r_y[ti]])
                if j == 7:
                    epi.tile(stiles[ti], [(yacc[:, ti, 0:512], r_y[ti]), (yacc[:, ti, 512:1024], r_y[ti])])
                    if ti % 4 == 3:
                        epi.flush()
            epi.flush()
            P.barrier()


def phase_moe_sparse(P, Dm, C, layer, src, dst, nxt, bg):
    phase_wtable(P, Dm, C, layer, bg)
    NB = phase_dispatch(P, Dm, C, layer)
    phase_blocks(P, Dm, C, layer, NB)
    phase_combine(P, Dm, C, layer, src, dst, nxt)


def build(layers, first_src="xin"):
    nc = bass.Bass("TRN2", target_bir_lowering=False)
    Dm = {}

    def din(name, shape, dt=F32):
        Dm[name] = nc.dram_tensor(name, list(shape), dt, kind="ExternalInput").ap()

    def dint(name, shape, dt=F32):
        Dm[name] = nc.dram_tensor(name, list(shape), dt, kind="Internal").ap()

    din("xin", [NTOK, 1024])
    din("ccT", [128, 32])
    for i in layers:
        kind = i % 3
        din("ada_w%d" % i, [1024, 6144]); din("ada_b%d" % i, [1, 6144])
        din("ln_g%d" % i, [2, 1024]); din("ln_b%d" % i, [2, 1024])
        if kind == 0:
            din("conf_w1_%d" % i, [1024, 2048]); din("conf_b1T_%d" % i, [128, 16])
            din("conf_dwT_%d" % i, [128, 8 * 31]); din("conf_pvec_%d" % i, [128, 24])
            din("conf_w2_%d" % i, [1024, 1024]); din("conf_b2_%d" % i, [1, 1024])
        elif kind == 1:
            din("sc_w_in_%d" % i, [1024, 3072]); din("sc_dwT_%d" % i, [128, 8 * 3]); din("sc_w_out_%d" % i, [1024, 1024])
        else:
            din("mla_w_dqkv_%d" % i, [1024, 704]); din("mla_gT_%d" % i, [128, 5])
            din("mla_w_uq_%d" % i, [384, 1536]); din("mla_w_uk_%d" % i, [256, 1024]); din("mla_w_uv_%d" % i, [256, 1024])
            din("mla_w_o_%d" % i, [1024, 1024])
            if "ropeCS" not in Dm:
                din("ropeCS", [64, 2, SEQ])
        din("moe_router%d" % i, [1024, 64]); din("moe_bias%d" % i, [1, 64])
        din("moe_w1_%d" % i, [64, 1024, 256]); din("moe_w3_%d" % i, [64, 1024, 256]); din("moe_w2_%d" % i, [64, 256, 1024])
        din("sh_w1_%d" % i, [1024, 256]); din("sh_w3_%d" % i, [1024, 256]); din("sh_w2_%d" % i, [256, 1024])
        dint("modrow%d" % i, [4, 6144])
    Dm["xout"] = nc.dram_tensor("xout", [NTOK, 1024], F32, kind="ExternalOutput").ap()
    dint("xresA", [NTOK, 1024]); dint("xresB", [NTOK, 1024])
    dint("hA", [128, 8, NTOK], BF16); dint("hB", [128, 8, NTOK], BF16)
    dint("gates", [NTOK, 64])
    dint("hrow", [NTOK, 1024], BF16)
    NSLOT = (36 * 8 + 64) * 128
    dint("hbuf", [NSLOT, 1024], BF16); dint("ybuf", [NSLOT, 1024]); dint("WS0", [64 * 128, WROW], BF16); dint("WS1", [64 * 128, WROW], BF16)
    dint("idxw", [128, 36 * 8 + 64], I32); dint("d8i", [NTOK, 8], I32); dint("g8", [NTOK, 8])

    P = Prog(nc)
    C = Consts()
    with P.gstack:
        phase_consts(P, C)
        phase_mod(P, Dm, C, layers)
        phase_prologue(P, Dm, C, layers[0])
        src = Dm["xin"]
        for li, i in enumerate(layers):
            lastl = li == len(layers) - 1
            kind = i % 3
            P.new_epoch()
            bg = BgW(P, Dm, i)
            if kind in (0, 1):
                phase_convmix(P, Dm, C, i, src, Dm["xresA"], (i, 2), bg)
            else:
                phase_mla(P, Dm, C, i, src, Dm["xresA"], (i, 2), bg)
            P.new_epoch()
            nxt = None if lastl else (layers[li + 1], 1)
            phase_moe_sparse(P, Dm, C, i, Dm["xresA"], Dm["xout"] if lastl else Dm["xresB"], nxt, bg)
            src = Dm["xresB"]
        if not has_ctx(layers[-1]):
            with P.phase():
                cp = P.sb("cp", [128, 2, 1024]); r_cp = Res()
                for b in range(2):
                    t0 = b * BSTR + SEQ
                    P.dma("sp", cp[:], src_last(Dm, layers)[t0:t0 + 256, :].rearrange("(n p) d -> p n d", p=128), W=[r_cp])
                    P.dma("sp", Dm["xout"][t0:t0 + 256, :].rearrange("(n p) d -> p n d", p=128), cp[:], R=[r_cp])
        P.barrier()
    return nc


def src_last(Dm, layers):
    cands = [i for i in layers if has_ctx(i)]
    if not cands:
        return Dm["xin"]
    return Dm["xresB"] if cands[-1] != layers[-1] else Dm["xout"]


def host_inputs(inputs, layers, xin_cores):
    f = lambda a: np.ascontiguousarray(np.asarray(a, dtype=np.float32))
    shared = {}
    for i in layers:
        kind, j = i % 3, i // 3
        shared["ada_w%d" % i] = f(inputs["ada_w"][i]); shared["ada_b%d" % i] = f(inputs["ada_b"][i][None, :])
        shared["ln_g%d" % i] = f(inputs["ln_g"][i]); shared["ln_b%d" % i] = f(inputs["ln_b"][i])
        if kind == 0:
            shared["conf_w1_%d" % i] = f(inputs["conf_w1"][j])
            shared["conf_b1T_%d" % i] = f(np.asarray(inputs["conf_b1"][j]).reshape(16, 128).T)
            shared["conf_dwT_%d" % i] = f(np.asarray(inputs["conf_dw"][j]).reshape(31, 8, 128).transpose(2, 1, 0).reshape(128, 248))
            pv = np.stack([np.asarray(inputs["conf_dwb"][j]).reshape(8, 128).T,
                           np.asarray(inputs["conf_ng"][j]).reshape(8, 128).T,
                           np.asarray(inputs["conf_nb"][j]).reshape(8, 128).T], axis=1)
            shared["conf_pvec_%d" % i] = f(pv.reshape(128, 24))
            shared["conf_w2_%d" % i] = f(inputs["conf_w2"][j]); shared["conf_b2_%d" % i] = f(inputs["conf_b2"][j][None, :])
        elif kind == 1:
            shared["sc_w_in_%d" % i] = f(inputs["sc_w_in"][j])
            shared["sc_dwT_%d" % i] = f(np.asarray(inputs["sc_dw"][j]).reshape(3, 8, 128).transpose(2, 1, 0).reshape(128, 24))
            shared["sc_w_out_%d" % i] = f(inputs["sc_w_out"][j])
        else:
            shared["mla_w_dqkv_%d" % i] = f(inputs["mla_w_dqkv"][j])
            gT = np.concatenate([np.asarray(inputs["mla_q_g"][j]).reshape(3, 128).T,
                                 np.asarray(inputs["mla_kv_g"][j]).reshape(2, 128).T], axis=1)
            shared["mla_gT_%d" % i] = f(gT)
            shared["mla_w_uq_%d" % i] = f(inputs["mla_w_uq"][j]); shared["mla_w_uk_%d" % i] = f(inputs["mla_w_uk"][j])
            shared["mla_w_uv_%d" % i] = f(inputs["mla_w_uv"][j]); shared["mla_w_o_%d" % i] = f(inputs["mla_w_o"][j])
            shared["ropeCS"] = rope_tables()
        shared["moe_router%d" % i] = f(inputs["moe_router"][i]); shared["moe_bias%d" % i] = f(inputs["moe_bias"][i][None, :])
        shared["moe_w1_%d" % i] = f(inputs["moe_w1"][i]); shared["moe_w3_%d" % i] = f(inputs["moe_w3"][i])
        shared["moe_w2_%d" % i] = f(inputs["moe_w2"][i])
        shared["sh_w1_%d" % i] = f(inputs["sh_w1"][i]); shared["sh_w3_%d" % i] = f(inputs["sh_w3"][i])
        shared["sh_w2_%d" % i] = f(inputs["sh_w2"][i])
    maps = []
    c = np.asarray(inputs["c"], dtype=np.float32)
    cctx = np.asarray(inputs["c_ctx"], dtype=np.float32)
    for core in range(NCORES):
        cc = np.zeros((4, 1024), np.float32)
        cc[0] = c[2 * core]; cc[1] = c[2 * core + 1]; cc[2] = cctx
        ccT = cc.reshape(4, 8, 128).transpose(2, 1, 0).reshape(128, 32)
        m = dict(shared)
        m["ccT"] = f(ccT)
        m["xin"] = xin_cores[core]
        maps.append(m)
    return maps


def rope_tables():
    n_freq = 16
    inv_freq = (10000.0 ** (-np.arange(n_freq, dtype=np.float32) / n_freq)).astype(np.float32)
    t = np.arange(SEQ)
    r = (t // 64).astype(np.float32)
    col = (t % 64).astype(np.float32)
    ang = np.concatenate([r[:, None] * inv_freq, col[:, None] * inv_freq], -1).astype(np.float32)
    cos = np.cos(ang).astype(np.float32).T
    sin = np.sin(ang).astype(np.float32).T
    out = np.empty((64, 2, SEQ), np.float32)
    out[0:32, 0] = cos; out[32:64, 0] = cos
    out[0:32, 1] = -sin; out[32:64, 1] = sin
    return np.ascontiguousarray(out)


def pack_x(inputs):
    x = np.asarray(inputs["x"], dtype=np.float32)
    ctx = np.asarray(inputs["ctx"], dtype=np.float32)
    out = []
    for core in range(NCORES):
        xin = np.empty((NTOK, 1024), np.float32)
        for b in range(2):
            xin[b * BSTR:b * BSTR + SEQ] = x[2 * core + b]
            xin[b * BSTR + SEQ:(b + 1) * BSTR] = ctx[2 * core + b]
        out.append(xin)
    return out


LAUNCH_GROUPS = [[0, 1, 2, 3]]
_NC_CACHE = {}


def run_groups(inputs, groups, core_ids=None):
    xin = pack_x(inputs)
    for layers in groups:
        key = tuple(layers)
        if key not in _NC_CACHE:
            _NC_CACHE[key] = build(layers)
        nc = _NC_CACHE[key]
        maps = host_inputs(inputs, layers, xin)
        if core_ids is not None:
            maps = [maps[c] for c in core_ids]
        res = run_bass_kernel_spmd(nc, maps, core_ids=list(range(len(maps))))
        outs = [np.asarray(r["xout"]) for r in res.results]
        if core_ids is not None:
            for k, c in enumerate(core_ids):
                xin[c] = outs[k]
        else:
            xin = outs
    return xin


def kernel(**inputs):
    xs = run_groups(inputs, LAUNCH_GROUPS)
    out = np.empty((16, SEQ, 1024), np.float32)
    for core in range(NCORES):
        for b in range(2):
            out[2 * core + b] = xs[core][b * BSTR:b * BSTR + SEQ]
    return out
```

```python
import contextlib
import numpy as np
import concourse.bass as bass
import concourse.mybir as mybir
from concourse.bass_utils import run_bass_kernel_spmd

F32 = mybir.dt.float32
BF16 = mybir.dt.bfloat16
AF = mybir.ActivationFunctionType
ALU = mybir.AluOpType
AX = mybir.AxisListType

SAME_ENGINE_SYNC = True
SPARSE = True
DEPTH = 4
ALPHA = (2.0 * DEPTH) ** 0.25
LN_EPS = 1e-5
RMS_EPS = 1e-6
NTOK = 4608
SEQ = 2048
CTX = 256
BSTR = SEQ + CTX
NCORES = 8


class Tok:
    __slots__ = ("sem", "val", "eng")

    def __init__(self, sem, val, eng):
        self.sem, self.val, self.eng = sem, val, eng


class Res:
    __slots__ = ("w", "r", "name")

    def __init__(self, name=""):
        self.w = None
        self.r = {}
        self.name = name


class Prog:
    ENG = ("pe", "act", "dve", "pool", "sp")

    def __init__(self, nc, n_dma_sems=24):
        self.nc = nc
        self.eng = {"pe": nc.tensor, "act": nc.scalar, "dve": nc.vector,
                    "pool": nc.gpsimd, "sp": nc.sync}
        self.gstack = contextlib.ExitStack()
        self.esem = {}
        self.pending = {e: [] for e in self.ENG}
        self.last = {e: None for e in self.ENG}
        self.waited = {}
        self.nsem = 0
        self.dma_pool = []
        for i in range(n_dma_sems):
            h = self.gstack.enter_context(nc.semaphore("dq%d" % i))
            self.dma_pool.append([h, 0, None])
        self.dma_rr = 0
        self.new_epoch()
        self.pstack = None
        self.uid = 0

    def new_epoch(self):
        for e in self.ENG:
            assert not self.pending[e], "pending unsignaled ops on %s at epoch change" % e
            self.nsem += 1
            h = self.gstack.enter_context(self.nc.semaphore("e%s%d" % (e, self.nsem)))
            self.esem[e] = [h, 0]

    def _wait(self, e, tok, force=False):
        if tok is None:
            return
        if tok.eng == e and not (force or (SAME_ENGINE_SYNC and e != "pe")):
            return
        assert tok.sem is not None, "dependency on an unsignaled op (engine %s)" % tok.eng
        key = (e, id(tok.sem))
        if self.waited.get(key, -1) >= tok.val:
            return
        self.eng[e].wait_ge(tok.sem, tok.val)
        self.waited[key] = tok.val

    def _deps(self, e, R, W, force=False):
        for r in R:
            self._wait(e, r.w, force)
        for w in W:
            self._wait(e, w.w, force)
            for t in w.r.values():
                self._wait(e, t, force)

    def _update(self, tok, R, W, key):
        for w in W:
            w.w = tok
            w.r = {}
        for r in R:
            r.r[key] = tok

    def op(self, e, fn, R=(), W=(), sig=True):
        self._deps(e, R, W)
        ins = fn(self.eng[e])
        if sig:
            s = self.esem[e]
            s[1] += 1
            ins.then_inc(s[0], 1)
            tok = Tok(s[0], s[1], e)
            for p in self.pending[e]:
                p.sem, p.val = tok.sem, tok.val
            self.pending[e] = []
        else:
            tok = Tok(None, None, e)
            self.pending[e].append(tok)
        self.last[e] = tok
        self._update(tok, R, W, e)
        return tok

    def dma(self, q, out, in_, R=(), W=(), **kw):
        slot = self.dma_pool[self.dma_rr]
        self.dma_rr = (self.dma_rr + 1) % len(self.dma_pool)
        self._wait(q, slot[2], True)
        self._deps(q, R, W, True)
        ins = self.eng[q].dma_start(out=out, in_=in_, **kw)
        slot[1] += 16
        ins.then_inc(slot[0], 16)
        tok = Tok(slot[0], slot[1], None)
        slot[2] = tok
        self.uid += 1
        self._update(tok, R, W, "d%d" % self.uid)
        return tok

    def idma(self, out, out_off, in_, in_off, R=(), W=()):
        q = "pool"
        slot = self.dma_pool[self.dma_rr]
        self.dma_rr = (self.dma_rr + 1) % len(self.dma_pool)
        self._wait(q, slot[2], True)
        self._deps(q, R, W, True)
        ins = self.eng[q].indirect_dma_start(out=out, out_offset=out_off, in_=in_, in_offset=in_off)
        slot[1] += 16
        ins.then_inc(slot[0], 16)
        tok = Tok(slot[0], slot[1], None)
        slot[2] = tok
        self.uid += 1
        self._update(tok, R, W, "d%d" % self.uid)
        return tok

    def barrier(self):
        toks = []
        for e in self.ENG:
            assert not self.pending[e], "pending unsignaled ops on %s at barrier" % e
            if self.last[e] is not None:
                toks.append(self.last[e])
        for slot in self.dma_pool:
            if slot[2] is not None:
                toks.append(slot[2])
        for e in self.ENG:
            for t in toks:
                self._wait(e, t, True)

    def phase(self):
        self.pstack = contextlib.ExitStack()
        return self.pstack

    def sb(self, name, shape, dt=F32, glob=False):
        st = self.gstack if glob else self.pstack
        self.uid += 1
        return st.enter_context(self.nc.sbuf_tensor("%s_%d" % (name, self.uid), list(shape), dt))

    def ps(self, name, shape, dt=F32, glob=False):
        st = self.gstack if glob else self.pstack
        self.uid += 1
        return st.enter_context(self.nc.psum_tensor("%s_%d" % (name, self.uid), list(shape), dt))

    def tt(self, e, out, a, b, op, R, W):
        return self.op(e, lambda E: E.tensor_tensor(out=out, in0=a, in1=b, op=op), R, W)

    def ts(self, e, out, a, s1, s2, op0, op1, R, W):
        if s2 is None:
            return self.op(e, lambda E: E.tensor_scalar(out=out, in0=a, scalar1=s1, scalar2=None, op0=op0), R, W)
        return self.op(e, lambda E: E.tensor_scalar(out=out, in0=a, scalar1=s1, scalar2=s2, op0=op0, op1=op1), R, W)

    def actv(self, out, in_, func, R, W, bias=None, scale=None):
        kw = {}
        if bias is not None:
            kw["bias"] = bias
        if scale is not None:
            kw["scale"] = scale
        return self.op("act", lambda E: E.activation(out=out, in_=in_, func=func, **kw), R, W)

    def mm(self, out, lhsT, rhs, start, stop, R, W, sig=None):
        if sig is None:
            sig = stop
        return self.op("pe", lambda E: E.matmul(out, lhsT=lhsT, rhs=rhs, start=start, stop=stop), R, W, sig=sig)


def has_ctx(i):
    return i < 2


def active_runs(i):
    if has_ctx(i):
        return [(0, NTOK)]
    return [(0, SEQ), (BSTR, SEQ)]


def tile_row(t0):
    b, r = divmod(t0, BSTR)
    return 2 if r >= SEQ else b


class Consts:
    pass


class Epi:
    def __init__(self, P, Dm, C, layer, which, nxt, router, src, dst, hdst, psT, r_psT, pslog=None, r_pslog=None,
                 ns=4):
        self.ns = ns
        self.P, self.Dm, self.C = P, Dm, C
        self.layer, self.which, self.nxt, self.router = layer, which, nxt, router
        self.src, self.dst, self.hdst = src, dst, hdst
        self.psT, self.r_psT, self.pslog, self.r_pslog = psT, r_psT, pslog, r_pslog
        self.row = None
        self.q = []
        sb = P.sb
        self.G = sb("G", [128, 1024]); self.rG = Res()
        self.LG = sb("LG", [128, 1024]); self.LB = sb("LB", [128, 1024]); self.rL = Res()
        if nxt is not None:
            self.LGA = sb("LGA", [128, 1024]); self.LBAS = sb("LBAS", [128, 1024]); self.rAS = Res()
        self.xt = [sb("xt", [128, 1024]) for _ in range(ns)]; self.rxt = [Res() for _ in range(ns)]
        self.z = [sb("z", [128, 1024]) for _ in range(ns)]; self.rz = [Res() for _ in range(ns)]
        self.st = sb("st", [128, ns, 2, 6]); self.rst = [Res() for _ in range(ns)]
        self.mv = sb("mv", [128, ns, 4]); self.rmv = Res()
        if nxt is not None:
            self.hTb = sb("hTb", [128, 8, ns * 128], BF16); self.rhTb = Res()
            if router:
                self.nh32 = 2 if ns > 2 else 1
                self.hT32 = [sb("hT32", [128, 8, 128]) for _ in range(self.nh32)]; self.rhT32 = [Res(), Res()]
        lg = Dm["ln_g%d" % layer][which - 1:which, :]
        lb = Dm["ln_b%d" % layer][which - 1:which, :]
        P.dma("sp", self.LG[:], lg.broadcast_to([128, 1024]), W=[self.rL])
        P.dma("sp", self.LB[:], lb.broadcast_to([128, 1024]), W=[self.rL])
        if router:
            self.wr = sb("wr", [128, 8, 64]); self.rwr = Res()
            P.dma("sp", self.wr[:], Dm["moe_router%d" % layer].rearrange("(k p) e -> p k e", p=128), W=[self.rwr])
            self.rb = sb("rb", [128, ns, 64])
            for i in range(ns):
                P.dma("sp", self.rb[:, i, :], Dm["moe_bias%d" % layer].broadcast_to([128, 64]), W=[self.rwr])
            self.gt = {}
            for nm, w in (("s", 64), ("sel", 64), ("eq", 64), ("m1", 8), ("m2", 8), ("gs", 8),
                          ("t8", 8), ("gm", 8), ("pen", 8), ("msk", 64), ("t8e", 8),
                          ("den", 1), ("rden", 1)):
                self.gt[nm] = (sb("g_" + nm, [128, ns, w]), Res())
            self.gt["sel2"] = self.gt["eq"]
            for nm in ("selm", "gun", "gate"):
                self.gt[nm] = self.gt["msk"]

    def ensure_row(self, j):
        if self.row == j:
            return
        self.row = j
        P, Dm = self.P, self.Dm
        mr = Dm["modrow%d" % self.layer]
        off = 2048 if self.which == 1 else 5120
        P.dma("sp", self.G[:], mr[j:j + 1, off:off + 1024].broadcast_to([128, 1024]), W=[self.rG])
        if self.nxt is not None:
            nl, nw = self.nxt
            mr2 = Dm["modrow%d" % nl]
            so = 0 if nw == 1 else 3072
            P.dma("sp", self.LBAS[:], mr2[j:j + 1, so:so + 1024].broadcast_to([128, 1024]), W=[self.rAS])
            P.dma("sp", self.LGA[:], mr2[j:j + 1, so + 1024:so + 2048].broadcast_to([128, 1024]), W=[self.rAS])
            scr, rscr = self.xt[self.ns - 1], self.rxt[self.ns - 1]
            P.tt("pool", scr[:], self.LGA[:], self.LB[:], ALU.mult, [self.rAS, self.rL], [rscr])
            P.tt("pool", self.LBAS[:], self.LBAS[:], scr[:], ALU.add, [self.rAS, rscr], [self.rAS])
            P.tt("pool", self.LGA[:], self.LGA[:], self.LG[:], ALU.mult, [self.rAS, self.rL], [self.rAS])

    def tile(self, t0, ysrc):
        P = self.P
        j = tile_row(t0)
        if self.q and (len(self.q) == self.ns or j != self.row or t0 != self.q[-1] + 128):
            self.flush()
        self.ensure_row(j)
        i = len(self.q)
        self.q.append(t0)
        xt, rxt, z, rz = self.xt[i], self.rxt[i], self.z[i], self.rz[i]
        P.dma("sp", xt[:], self.src[t0:t0 + 128, :], W=[rxt])
        for hf in range(2):
            ap, res = ysrc[hf]
            P.tt("dve", z[:, hf * 512:(hf + 1) * 512], ap, self.G[:, hf * 512:(hf + 1) * 512], ALU.mult,
                 [res, self.rG], [rz])

    def flush(self):
        P, C = self.P, self.C
        q = self.q
        n = len(q)
        if n == 0:
            return
        self.q = []
        T0 = q[0]
        mv, rmv = self.mv, self.rmv
        for i in range(n):
            P.tt("dve", self.z[i][:], self.z[i][:], self.xt[i][:], ALU.add, [self.rz[i], self.rxt[i]], [self.rz[i]])
        for i in range(n):
            z, rz = self.z[i], self.rz[i]
            for hf in range(2):
                P.op("dve", lambda E, hf=hf, i=i, z=z: E.bn_stats(out=self.st[:, i, hf, :],
                                                              in_=z[:, hf * 512:(hf + 1) * 512]), [rz], [self.rst[i]])
            P.op("dve", lambda E, i=i: E.bn_aggr(out=mv[:, i, 0:2], in_=self.st[:, i, :, :].rearrange("p a b -> p (a b)")),
                 [self.rst[i]], [rmv])
        P.actv(mv[:, 0:n, 2:3], mv[:, 0:n, 1:2], AF.Sqrt, [rmv], [rmv], bias=C.eps_ln[:, 0:1], scale=1.0)
        P.op("dve", lambda E: E.reciprocal(out=mv[:, 0:n, 2:3], in_=mv[:, 0:n, 2:3]), [rmv], [rmv])
        P.op("dve", lambda E: E.scalar_tensor_tensor(out=mv[:, 0:n, 3:4], in0=mv[:, 0:n, 0:1], scalar=-1.0,
                                                     in1=mv[:, 0:n, 2:3], op0=ALU.mult, op1=ALU.mult), [rmv], [rmv])
        for i in range(n):
            z, rz = self.z[i], self.rz[i]
            P.actv(z[:], z[:], AF.Identity, [rz, rmv], [rz], bias=mv[:, i, 3:4], scale=mv[:, i, 2:3])
        if self.nxt is not None:
            for i in range(n):
                z, rz, h, rh = self.z[i], self.rz[i], self.xt[i], self.rxt[i]
                P.tt("dve", h[:], z[:], self.LGA[:], ALU.mult, [rz, self.rAS], [rh])
                P.tt("dve", h[:], h[:], self.LBAS[:], ALU.add, [rh, self.rAS], [rh])
        for i in range(n):
            z, rz = self.z[i], self.rz[i]
            P.tt("pool", z[:], z[:], self.LG[:], ALU.mult, [rz, self.rL], [rz])
            P.tt("pool", z[:], z[:], self.LB[:], ALU.add, [rz, self.rL], [rz])
            P.dma("sp", self.dst[q[i]:q[i] + 128, :], z[:], R=[rz])
        if self.nxt is None:
            return
        for i in range(n):
            h, rh = self.xt[i], self.rxt[i]
            for k in range(8):
                P.op("pe", lambda E, k=k, h=h: E.transpose(out=self.psT[k // 4][:, (k % 4) * 128:(k % 4 + 1) * 128],
                                                          in_=h[:, k * 128:(k + 1) * 128], identity=C.ident32[:]),
                     [rh, C.r_const], [self.r_psT[k // 4]], sig=(k % 4 == 3))
            hlo = self.hTb[:, 0:4, i * 128:(i + 1) * 128]
            hhi = self.hTb[:, 4:8, i * 128:(i + 1) * 128]
            p0 = self.psT[0][:].rearrange("p (k t) -> p k t", k=4)
            p1 = self.psT[1][:].rearrange("p (k t) -> p k t", k=4)
            if self.router:
                self.P.dma("pool", self.Dm["hrow"][q[i]:q[i] + 128, :], h[:], R=[rh])
                h32, rh32 = self.hT32[i % self.nh32], self.rhT32[i % self.nh32]
                P.op("act", lambda E, h32=h32, p0=p0: E.copy(out=h32[:, 0:4, :], in_=p0), [self.r_psT[0]], [rh32])
                P.op("act", lambda E, hlo=hlo, p0=p0: E.copy(out=hlo, in_=p0), [self.r_psT[0]], [self.rhTb])
                P.op("dve", lambda E, h32=h32, p1=p1: E.tensor_copy(out=h32[:, 4:8, :], in_=p1), [self.r_psT[1]], [rh32])
                P.op("dve", lambda E, hhi=hhi, p1=p1: E.tensor_copy(out=hhi, in_=p1), [self.r_psT[1]], [self.rhTb])
                for k in range(8):
                    P.mm(self.pslog[:, i * 64:(i + 1) * 64], h32[:, k, :], self.wr[:, k, :], k == 0, k == 7,
                         [rh32, self.rwr], [self.r_pslog])
            else:
                P.op("act", lambda E, hlo=hlo, p0=p0: E.copy(out=hlo, in_=p0), [self.r_psT[0]], [self.rhTb])
                P.op("dve", lambda E, hhi=hhi, p1=p1: E.tensor_copy(out=hhi, in_=p1), [self.r_psT[1]], [self.rhTb])
        P.dma("sp", self.hdst[:, :, T0:T0 + n * 128], self.hTb[:, :, 0:n * 128], R=[self.rhTb])
        if self.router:
            self.gating(T0, n)

    def gating(self, T0, n):
        P = self.P
        g = self.gt
        BIG = 1.0e4

        def T(nm):
            return g[nm][0][:, 0:n, :]

        def R(nm):
            return g[nm][1]

        def g3(ap):
            return ap.rearrange("p n (g e) -> p (n g) e", e=8)

        def f2(ap):
            return ap.rearrange("p n w -> p (n w)")
        pl = self.pslog[:, 0:n * 64].rearrange("p (n e) -> p n e", e=64)
        P.actv(T("s"), pl, AF.Sigmoid, [self.r_pslog], [R("s")])
        P.tt("dve", T("sel"), T("s"), self.rb[:, 0:n, :], ALU.add, [R("s"), self.rwr], [R("sel")])
        P.op("dve", lambda E: E.tensor_reduce(out=f2(T("m1")), in_=g3(T("sel")), axis=AX.X, op=ALU.max), [R("sel")], [R("m1")])
        m1b = f2(T("m1")).unsqueeze(2).broadcast_to([128, n * 8, 8])
        P.tt("dve", g3(T("eq")), g3(T("sel")), m1b, ALU.is_equal, [R("sel"), R("m1")], [R("eq")])
        P.op("dve", lambda E: E.scalar_tensor_tensor(out=f2(T("sel2")), in0=f2(T("eq")), scalar=-BIG, in1=f2(T("sel")),
                                                     op0=ALU.mult, op1=ALU.add), [R("eq"), R("sel")], [R("sel2")])
        P.op("dve", lambda E: E.tensor_reduce(out=f2(T("m2")), in_=g3(T("sel2")), axis=AX.X, op=ALU.max), [R("sel2")], [R("m2")])
        P.tt("dve", T("gs"), T("m1"), T("m2"), ALU.add, [R("m1"), R("m2")], [R("gs")])
        for i in range(n):
            P.op("dve", lambda E, i=i: E.max(out=g["t8"][0][:, i, :], in_=g["gs"][0][:, i, :]), [R("gs")], [R("t8")])
        thr = g["t8"][0][:, 0:n, 3:4].broadcast_to([128, n, 8])
        P.tt("dve", T("gm"), T("gs"), thr, ALU.is_ge, [R("gs"), R("t8")], [R("gm")])
        P.ts("dve", f2(T("pen")), f2(T("gm")), -1.0, BIG, ALU.add, ALU.mult, [R("gm")], [R("pen")])
        gmb = f2(T("gm")).unsqueeze(2).broadcast_to([128, n * 8, 8])
        penb = f2(T("pen")).unsqueeze(2).broadcast_to([128, n * 8, 8])
        P.tt("dve", g3(T("msk")), g3(T("sel")), gmb, ALU.mult, [R("sel"), R("gm")], [R("msk")])
        P.tt("dve", g3(T("msk")), g3(T("msk")), penb, ALU.add, [R("msk"), R("pen")], [R("msk")])
        for i in range(n):
            P.op("dve", lambda E, i=i: E.max(out=g["t8e"][0][:, i, :], in_=g["msk"][0][:, i, :]), [R("msk")], [R("t8e")])
        thr8 = g["t8e"][0][:, 0:n, 7:8].broadcast_to([128, n, 64])
        P.tt("dve", T("selm"), T("msk"), thr8, ALU.is_ge, [R("msk"), R("t8e")], [R("selm")])
        P.tt("dve", T("gun"), T("s"), T("selm"), ALU.mult, [R("s"), R("selm")], [R("gun")])
        P.op("dve", lambda E: E.tensor_reduce(out=f2(T("den")), in_=T("gun"), axis=AX.X, op=ALU.add), [R("gun")], [R("den")])
        P.op("dve", lambda E: E.reciprocal(out=T("rden"), in_=T("den")), [R("den")], [R("rden")])
        rdb = g["rden"][0][:, 0:n, 0:1].broadcast_to([128, n, 64])
        P.op("dve", lambda E: E.scalar_tensor_tensor(out=T("gate"), in0=T("gun"), scalar=2.5, in1=rdb,
                                                     op0=ALU.mult, op1=ALU.mult), [R("gun"), R("rden")], [R("gate")])
        P.dma("sp", self.Dm["gates"][T0:T0 + n * 128, :].rearrange("(n p) e -> p n e", p=128), T("gate"), R=[R("gate")])


def phase_consts(P, C):
    C.r_const = Res()
    C.ident32 = P.sb("ident32", [128, 128], F32, glob=True)
    C.identb = P.sb("identb", [128, 128], BF16, glob=True)
    C.ones_avg = P.sb("ones_avg", [128, 128], F32, glob=True)
    P.op("pool", lambda e: e.memset(C.ident32[:], 1.0), W=[C.r_const])
    P.op("pool", lambda e: e.affine_select(out=C.ident32[:], in_=C.ident32[:], pattern=[[-1, 128]],
                                           compare_op=ALU.is_equal, fill=0.0, base=0, channel_multiplier=1),
         R=[C.r_const], W=[C.r_const])
    P.op("pool", lambda e: e.tensor_copy(out=C.identb[:], in_=C.ident32[:]), R=[C.r_const], W=[C.r_const])
    P.op("pool", lambda e: e.memset(C.ones_avg[:], 1.0 / 1024.0), W=[C.r_const])
    C.eps_ln = P.sb("eps_ln", [128, 4], F32, glob=True)
    P.op("pool", lambda e: e.memset(C.eps_ln[:, 0:1], LN_EPS / (ALPHA * ALPHA)), W=[C.r_const])
    P.op("pool", lambda e: e.memset(C.eps_ln[:, 1:2], LN_EPS), W=[C.r_const])
    P.op("pool", lambda e: e.memset(C.eps_ln[:, 2:3], RMS_EPS), W=[C.r_const])


def phase_mod(P, Dm, C, layers):
    with P.phase():
        ccT = P.sb("ccT", [128, 8, 4]); r_cc = Res()
        sT = P.sb("sT", [128, 8, 4]); r_sT = Res()
        P.dma("sp", ccT[:].rearrange("p k j -> p (k j)"), Dm["ccT"], W=[r_cc])
        P.actv(sT[:], ccT[:], AF.Silu, [r_cc], [r_sT])
        wt = [P.sb("adaw", [128, 8, 512]) for _ in range(2)]; r_wt = [Res(), Res()]
        adab = P.sb("adab", [4, 6144]); r_ab = Res()
        msb = P.sb("msb", [4, 6144]); r_ms = Res()
        ps = [P.ps("psm", [128, 512]) for _ in range(2)]; r_ps = [Res(), Res()]
        n = 0
        for i in layers:
            P.dma("sp", adab[:], Dm["ada_b%d" % i].broadcast_to([4, 6144]), W=[r_ab])
            for nt in range(12):
                b = n % 2
                n += 1
                P.dma("sp" if nt % 2 == 0 else "act", wt[b][:],
                      Dm["ada_w%d" % i][:, nt * 512:(nt + 1) * 512].rearrange("(k p) f -> p k f", p=128), W=[r_wt[b]])
                for k in range(8):
                    P.mm(ps[b][0:4, :], sT[:, k, :], wt[b][:, k, :], k == 0, k == 7, [r_sT, r_wt[b]], [r_ps[b]])
                P.tt("dve", msb[:, nt * 512:(nt + 1) * 512], ps[b][0:4, :], adab[:, nt * 512:(nt + 1) * 512],
                     ALU.add, [r_ps[b], r_ab], [r_ms])
            for off in (1024, 4096):
                P.ts("dve", msb[:, off:off + 1024], msb[:, off:off + 1024], 1.0, None, ALU.add, None, [r_ms], [r_ms])
            for off in (2048, 5120):
                P.ts("dve", msb[:, off:off + 1024], msb[:, off:off + 1024], 1.0 / ALPHA, None, ALU.mult, None,
                     [r_ms], [r_ms])
            P.dma("sp", Dm["modrow%d" % i], msb[:], R=[r_ms])
        P.barrier()


def phase_prologue(P, Dm, C, layer):
    with P.phase():
        A = P.sb("A", [128, 1024]); S = P.sb("S", [128, 1024]); rAS = Res()
        xt = [P.sb("xt", [128, 1024]) for _ in range(2)]; rxt = [Res(), Res()]
        h = [P.sb("h", [128, 1024]) for _ in range(2)]; rh = [Res(), Res()]
        hTb = [P.sb("hTb", [128, 8, 128], BF16) for _ in range(2)]; rhTb = [Res(), Res()]
        psT = [P.ps("psT", [128, 512]) for _ in range(2)]; r_psT = [Res(), Res()]
        mr = Dm["modrow%d" % layer]
        row = None
        n = 0
        runs = [(0, NTOK)] if layer <= 2 else active_runs(layer)
        for (s0, ln) in runs:
            for t0 in range(s0, s0 + ln, 128):
                j = tile_row(t0)
                if j != row:
                    row = j
                    P.dma("sp", S[:], mr[j:j + 1, 0:1024].broadcast_to([128, 1024]), W=[rAS])
                    P.dma("sp", A[:], mr[j:j + 1, 1024:2048].broadcast_to([128, 1024]), W=[rAS])
                b = n % 2
                n += 1
                P.dma("sp", xt[b][:], Dm["xin"][t0:t0 + 128, :], W=[rxt[b]])
                P.tt("pool", h[b][:], xt[b][:], A[:], ALU.mult, [rxt[b], rAS], [rh[b]])
                P.tt("dve", h[b][:], h[b][:], S[:], ALU.add, [rh[b], rAS], [rh[b]])
                for k in range(8):
                    P.op("pe", lambda E, k=k, b=b: E.transpose(out=psT[k // 4][:, (k % 4) * 128:(k % 4 + 1) * 128],
                                                               in_=h[b][:, k * 128:(k + 1) * 128], identity=C.ident32[:]),
                         [rh[b], C.r_const], [r_psT[k // 4]], sig=(k % 4 == 3))
                P.op("act", lambda E, b=b: E.copy(out=hTb[b][:, 0:4, :].rearrange("p k t -> p (k t)"), in_=psT[0][:]),
                     [r_psT[0]], [rhTb[b]])
                P.op("dve", lambda E, b=b: E.tensor_copy(out=hTb[b][:, 4:8, :].rearrange("p k t -> p (k t)"),
                                                         in_=psT[1][:]), [r_psT[1]], [rhTb[b]])
                P.dma("sp", Dm["hA"][:, :, t0:t0 + 128], hTb[b][:], R=[rhTb[b]])
        P.barrier()


def phase_convmix(P, Dm, C, layer, src, dst, nxt, bg):
    kind, j = layer % 3, layer // 3
    KW = 31 if kind == 0 else 3
    PAD = (KW - 1) // 2
    ctx_on = has_ctx(layer)
    NW1 = 2048 if kind == 0 else 3072
    w1name = "conf_w1_%d" % layer if kind == 0 else "sc_w_in_%d" % layer
    w2name = "conf_w2_%d" % layer if kind == 0 else "sc_w_out_%d" % layer
    dwname = "conf_dwT_%d" % layer if kind == 0 else "sc_dwT_%d" % layer
    VLEN = (SEQ + 2 * PAD) + (CTX + 2 * PAD)
    layer_stack = contextlib.ExitStack()
    P.uid += 1
    diag = layer_stack.enter_context(P.nc.sbuf_tensor("diag_%d" % P.uid, [128, 8 * KW, 128], BF16)); r_diag = Res()
    dwT = layer_stack.enter_context(P.nc.sbuf_tensor("dwT_%d" % P.uid, [128, 8, KW], F32)); r_dw = Res()
    P.dma("sp", dwT[:].rearrange("p c k -> p (c k)"), Dm[dwname], W=[r_dw])
    for c in range(8):
        for k in range(KW):
            P.ts("dve", diag[:, c * KW + k, :], C.identb[:], dwT[:, c, k:k + 1], None, ALU.mult, None,
                 [C.r_const, r_dw], [r_diag])
    for b in range(2):
        seqs = [(b * BSTR, SEQ, 0)]
        if ctx_on:
            seqs.append((b * BSTR + SEQ, CTX, SEQ + 2 * PAD))
        outer = contextlib.ExitStack()
        with outer:
            P.uid += 1
            vT = outer.enter_context(P.nc.sbuf_tensor("vT_%d" % P.uid, [128, 8, VLEN], BF16)); r_vT = Res()
            gbT = None
            if kind == 1:
                P.uid += 1
                gbT = outer.enter_context(P.nc.sbuf_tensor("gbT_%d" % P.uid, [128, 8, BSTR], BF16)); r_gbT = Res()
            with P.phase():
                bg.attach(3)
                P.op("pool", lambda E: E.memset(vT[:], 0.0), W=[r_vT])
                w1 = P.sb("w1", [128, 8, NW1], BF16); r_w1 = Res()
                for k in range(8):
                    P.dma("pool", w1[:, k, :], Dm[w1name][k * 128:(k + 1) * 128, :], W=[r_w1])
                if kind == 0:
                    b1T = P.sb("b1T", [128, 16]); r_b1 = Res()
                    P.dma("sp", b1T[:], Dm["conf_b1T_%d" % layer], W=[r_b1])
                hT = [P.sb("hT", [128, 8, 512], BF16) for _ in range(2)]; r_hT = [Res(), Res()]
                sg = [P.sb("sg", [128, 512]) for _ in range(2)]; r_sg = [Res(), Res()]
                NB = 3 if kind == 1 else 2
                psa = [[P.ps("psa", [128, 512]) for _ in range(NB)] for _ in range(2)]
                r_psa = [[Res() for _ in range(NB)] for _ in range(2)]
                n = 0
                m = 0
                for (s0, ln, voff) in seqs:
                    for t0 in range(0, ln, 512):
                        N = min(512, ln - t0)
                        hb = n % 2
                        n += 1
                        P.dma("sp", hT[hb][:, :, 0:N], Dm["hA"][:, :, s0 + t0:s0 + t0 + N], W=[r_hT[hb]])
                        for fc in range(8):
                            bg.step(3)
                            pb = m % 2
                            m += 1
                            for w in range(NB):
                                col = w * 1024 + fc * 128
                                for k in range(8):
                                    P.mm(psa[pb][w][:, 0:N], w1[:, k, col:col + 128], hT[hb][:, k, 0:N], k == 0, k == 7,
                                         [r_w1, r_hT[hb]], [r_psa[pb][w]])
                            vdst = vT[:, fc, voff + PAD + t0:voff + PAD + t0 + N]
                            if kind == 0:
                                P.actv(sg[pb][:, 0:N], psa[pb][1][:, 0:N], AF.Sigmoid, [r_psa[pb][1], r_b1], [r_sg[pb]],
                                       bias=b1T[:, 8 + fc:9 + fc])
                                P.op("dve", lambda E, pb=pb, N=N, fc=fc, vdst=vdst: E.scalar_tensor_tensor(
                                    out=vdst, in0=psa[pb][0][:, 0:N], scalar=b1T[:, fc:fc + 1], in1=sg[pb][:, 0:N],
                                    op0=ALU.add, op1=ALU.mult), [r_psa[pb][0], r_sg[pb], r_b1], [r_vT])
                            else:
                                P.op("act", lambda E, pb=pb, N=N, fc=fc, s0=s0, t0=t0: E.copy(
                                    out=gbT[:, fc, s0 - b * BSTR + t0:s0 - b * BSTR + t0 + N], in_=psa[pb][0][:, 0:N]),
                                    [r_psa[pb][0]], [r_gbT])
                                P.op("act", lambda E, pb=pb, N=N: E.copy(out=sg[pb][:, 0:N], in_=psa[pb][1][:, 0:N]),
                                     [r_psa[pb][1]], [r_sg[pb]])
                                P.tt("dve", vdst, psa[pb][2][:, 0:N], sg[pb][:, 0:N], ALU.mult,
                                     [r_psa[pb][2], r_sg[pb]], [r_vT])
                bg.detach()
                P.barrier()
            with P.phase():
                w2 = P.sb("w2", [128, 8, 1024], BF16); r_w2 = Res()
                for k in range(8):
                    P.dma("pool", w2[:, k, :], Dm[w2name][k * 128:(k + 1) * 128, :], W=[r_w2])
                sT = [P.sb("sT", [128, 8, 512], BF16) for _ in range(1)]; r_sT = [Res(), Res()]
                psc = [P.ps("psc", [128, 512]) for _ in range(2)]; r_psc = [Res(), Res()]
                psy = [P.ps("psy", [128, 512]) for _ in range(2)]; r_psy = [Res(), Res()]
                psT = [P.ps("psT", [128, 512]) for _ in range(2)]; r_psT = [Res(), Res()]
                if kind == 0:
                    pvec = P.sb("pvec", [128, 3, 8]); r_pv = Res()
                    P.dma("sp", pvec[:].rearrange("p a c -> p (a c)"), Dm["conf_pvec_%d" % layer], W=[r_pv])
                    cv = P.sb("cv", [128, 8, 512]); r_cvc = [Res() for _ in range(8)]
                    sq = [P.sb("sq", [128, 512])] * 2; r_sq = [Res()] * 2
                    pss = [P.ps("pss", [128, 512]) for _ in range(2)]; r_pss = Res()
                    meanB = P.sb("meanB", [128, 512]); rstdB = P.sb("rstdB", [128, 512]); r_mr = Res()
                    pslog, r_pslog = pss[0], r_pss
                    r_b2 = Res()
                    b2f = cv[0:1, 0:2, :].rearrange("p a b -> p (a b)")
                    b2t = cv[0:1, 2:4, :].rearrange("p a b -> p (a b)")
                    rsc = [r_cvc[0], r_cvc[1], r_cvc[2], r_cvc[3]]
                    b2hl = P.sb("b2hl", [1, 2, 1024], BF16)
                    ones1 = P.sb("ones1", [1, 128], BF16)
                    P.op("pool", lambda E: E.memset(ones1[:], 1.0), W=[r_b2])
                    P.dma("sp", b2f, Dm["conf_b2_%d" % layer], W=rsc)
                    P.op("pool", lambda E: E.tensor_copy(out=b2hl[:, 0, :], in_=b2f), rsc, [r_b2])
                    P.op("pool", lambda E: E.tensor_copy(out=b2t, in_=b2hl[:, 0, :]), [r_b2], rsc)
                    P.tt("pool", b2t, b2f, b2t, ALU.subtract, rsc, rsc)
                    P.op("pool", lambda E: E.tensor_copy(out=b2hl[:, 1, :], in_=b2t), rsc, [r_b2])
                else:
                    pslog = P.ps("pslog", [128, 512]); r_pslog = Res()
                epi = Epi(P, Dm, C, layer, 1, nxt, True, src, dst, Dm["hB"], psT, r_psT, pslog, r_pslog,
                          ns=(2 if kind == 0 else 4))
                n = 0
                m = 0
                for (s0, ln, voff) in seqs:
                    for t0 in range(0, ln, 512):
                        N = min(512, ln - t0)
                        sb_ = 0
                        n += 1
                        for c in range(8):
                            pb = m % 2
                            m += 1
                            for k in range(KW):
                                P.mm(psc[pb][:, 0:N], diag[:, c * KW + k, :],
                                     vT[:, c, voff + t0 + k:voff + t0 + k + N], k == 0, k == KW - 1,
                                     [r_diag, r_vT], [r_psc[pb]])
                            if kind == 0:
                                P.actv(cv[:, c, 0:N], psc[pb][:, 0:N], AF.Identity, [r_psc[pb], r_pv], [r_cvc[c]],
                                       bias=pvec[:, 0, c:c + 1])
                                P.actv(sq[pb][:, 0:N], cv[:, c, 0:N], AF.Square, [r_cvc[c]], [r_sq[pb]])
                                P.mm(pss[0][:, 0:N], C.ones_avg[:], cv[:, c, 0:N], c == 0, c == 7,
                                     [C.r_const, r_cvc[c]], [r_pss], sig=True)
                                P.mm(pss[1][:, 0:N], C.ones_avg[:], sq[pb][:, 0:N], c == 0, c == 7,
                                     [C.r_const, r_sq[pb]], [r_pss], sig=True)
                            else:
                                P.tt("dve", sT[sb_][:, c, 0:N], psc[pb][:, 0:N],
                                     gbT[:, c, s0 - b * BSTR + t0:s0 - b * BSTR + t0 + N], ALU.mult,
                                     [r_psc[pb], r_gbT], [r_sT[sb_]])
                        if kind == 0:
                            P.op("act", lambda E, N=N: E.copy(out=meanB[:, 0:N], in_=pss[0][:, 0:N]), [r_pss], [r_mr])
                            P.actv(rstdB[:, 0:N], pss[0][:, 0:N], AF.Square, [r_pss], [r_mr])
                            P.tt("dve", rstdB[:, 0:N], pss[1][:, 0:N], rstdB[:, 0:N], ALU.subtract, [r_pss, r_mr], [r_mr])
                            P.actv(rstdB[:, 0:N], rstdB[:, 0:N], AF.Sqrt, [r_mr], [r_mr], bias=C.eps_ln[:, 1:2], scale=1.0)
                            P.op("dve", lambda E, N=N: E.reciprocal(out=rstdB[:, 0:N], in_=rstdB[:, 0:N]), [r_mr], [r_mr])
                            for c in range(8):
                                P.tt("pool", cv[:, c, 0:N], cv[:, c, 0:N], meanB[:, 0:N], ALU.subtract,
                                     [r_cvc[c], r_mr], [r_cvc[c]])
                                P.tt("dve", cv[:, c, 0:N], cv[:, c, 0:N], rstdB[:, 0:N], ALU.mult,
                                     [r_cvc[c], r_mr], [r_cvc[c]])
                                P.actv(sT[sb_][:, c, 0:N], cv[:, c, 0:N], AF.Silu, [r_cvc[c], r_pv], [r_sT[sb_]],
                                       bias=pvec[:, 2, c:c + 1], scale=pvec[:, 1, c:c + 1])
                        for jt in range(N // 128):
                            for hf in range(2):
                                for k in range(8):
                                    P.mm(psy[hf][:, :], sT[sb_][:, k, jt * 128:(jt + 1) * 128],
                                         w2[:, k, hf * 512:(hf + 1) * 512], k == 0, (k == 7 and kind != 0),
                                         [r_sT[sb_], r_w2], [r_psy[hf]])
                                if kind == 0:
                                    for hl in range(2):
                                        P.mm(psy[hf][:, :], ones1[:], b2hl[:, hl, hf * 512:(hf + 1) * 512], False, hl == 1,
                                             [r_b2], [r_psy[hf]])
                            epi.tile(s0 + t0 + jt * 128, [(psy[0][:, :], r_psy[0]), (psy[1][:, :], r_psy[1])])
                epi.flush()
                P.barrier()


    layer_stack.close()

ATTN_SCALE = 192.0 ** -0.5
NKV = BSTR
NKC = NKV // 128


def phase_mla(P, Dm, C, layer, src, dst, nxt, bg):
    L = layer
    for b in range(2):
        outer = contextlib.ExitStack()
        with outer:
            def osb(name, shape, dt):
                P.uid += 1
                return outer.enter_context(P.nc.sbuf_tensor("%s_%d" % (name, P.uid), list(shape), dt))
            cqn = osb("cqn", [128, 3, SEQ], BF16); r_cqn = Res()
            KnT = osb("KnT", [128, 8, NKV], BF16); r_KnT = Res()
            KpT = osb("KpT", [128, NKV], BF16); r_KpT = Res()
            V = osb("V", [128, NKC, 1024], BF16); r_V = Res()
            with P.phase():
                wd = P.sb("wd", [128, 8, 704], BF16); r_wd = Res()
                for k in range(8):
                    P.dma("pool", wd[:, k, :], Dm["mla_w_dqkv_%d" % L][k * 128:(k + 1) * 128, :], W=[r_wd])
                wsw = P.sb("wsw", [128, 8, 64], BF16)
                P.op("pool", lambda E: E.tensor_copy(out=wsw[:, :, 0:32], in_=wd[:, :, 672:704]), [r_wd], [r_wd])
                P.op("pool", lambda E: E.tensor_copy(out=wsw[:, :, 32:64], in_=wd[:, :, 640:672]), [r_wd], [r_wd])
                wuk = P.sb("wuk", [128, 2, 1024], BF16); wuv = P.sb("wuv", [128, 2, 1024], BF16); r_wu = Res()
                for k in range(2):
                    P.dma("pool", wuk[:, k, :], Dm["mla_w_uk_%d" % L][k * 128:(k + 1) * 128, :], W=[r_wu])
                    P.dma("pool", wuv[:, k, :], Dm["mla_w_uv_%d" % L][k * 128:(k + 1) * 128, :], W=[r_wu])
                bg.attach(3)
                gv = P.sb("gv", [128, 5]); r_gv = Res()
                P.dma("sp", gv[:], Dm["mla_gT_%d" % L], W=[r_gv])
                ones_q = P.sb("ones_q", [128, 128]); ones_kv = P.sb("ones_kv", [128, 128]); r_on = Res()
                P.op("pool", lambda E: E.memset(ones_q[:], 1.0 / 384.0), W=[r_on])
                P.op("pool", lambda E: E.memset(ones_kv[:], 1.0 / 256.0), W=[r_on])
                P.op("pool", lambda E: E.memset(KpT[64:65, :], 1.0), W=[r_KpT])
                hT = [P.sb("hT", [128, 8, 512], BF16) for _ in range(2)]; r_hT = [Res(), Res()]
                dsb = P.sb("dsb", [128, 6, 512]); r_dsb = Res()
                dsw = P.sb("dsw", [128, 512]); r_dsw = Res()
                sq = [P.sb("sq", [128, 512]) for _ in range(2)]; r_sq = [Res(), Res()]
                rq = P.sb("rq", [128, 512]); rkv = P.sb("rkv", [128, 512]); r_rr = Res()
                tmp = [P.sb("tmp", [128, 512]) for _ in range(2)]; r_tmp = [Res(), Res()]
                ckvn = P.sb("ckvn", [128, 2, 512], BF16); r_ckvn = Res()
                rope = P.sb("rope", [64, 2, 512]); r_rope = Res()
                psd = [P.ps("psd", [128, 512]) for _ in range(2)]; r_psd = [Res(), Res()]
                pss = [P.ps("pss", [128, 512]) for _ in range(2)]; r_pss = [Res(), Res()]
                psk = [P.ps("psk", [128, 512]) for _ in range(2)]; r_psk = [Res(), Res()]
                n = 0
                m = 0
                for t0 in range(0, NKV, 512):
                    N = min(512, NKV - t0)
                    isx = t0 < SEQ
                    hb = n % 2
                    n += 1
                    P.dma("sp", hT[hb][:, :, 0:N], Dm["hA"][:, :, b * BSTR + t0:b * BSTR + t0 + N], W=[r_hT[hb]])
                    if isx:
                        P.dma("sp", rope[:], Dm["ropeCS"][:, :, t0:t0 + 512], W=[r_rope])
                    for oc in range(7):
                        bg.step(2)
                        pb = m % 2
                        m += 1
                        M = 128 if oc < 5 else 64
                        for k in range(8):
                            lw = wd[:, k, oc * 128:oc * 128 + M] if oc < 6 else wsw[:, k, :]
                            P.mm(psd[pb][0:M, 0:N], lw, hT[hb][:, k, 0:N], k == 0, k == 7, [r_wd, r_hT[hb]], [r_psd[pb]])
                        if oc < 6:
                            P.op("act", lambda E, pb=pb, M=M, N=N, oc=oc: E.copy(out=dsb[0:M, oc, 0:N], in_=psd[pb][0:M, 0:N]),
                                 [r_psd[pb]], [r_dsb])
                        else:
                            P.op("act", lambda E, pb=pb, N=N: E.copy(out=dsw[0:64, 0:N], in_=psd[pb][0:64, 0:N]),
                                 [r_psd[pb]], [r_dsw])
                        if oc < 5:
                            grp = 0 if oc < 3 else 1
                            first = oc in (0, 3)
                            last = oc in (2, 4)
                            sb_ = oc % 2
                            P.actv(sq[sb_][:, 0:N], dsb[:, oc, 0:N], AF.Square, [r_dsb], [r_sq[sb_]])
                            P.mm(pss[grp][:, 0:N], (ones_q if grp == 0 else ones_kv)[:], sq[sb_][:, 0:N], first, last,
                                 [r_on, r_sq[sb_]], [r_pss[grp]], sig=True)
                    for grp, rr in ((0, rq), (1, rkv)):
                        P.actv(rr[:, 0:N], pss[grp][:, 0:N], AF.Sqrt, [r_pss[grp]], [r_rr], bias=C.eps_ln[:, 2:3], scale=1.0)
                        P.op("dve", lambda E, rr=rr, N=N: E.reciprocal(out=rr[:, 0:N], in_=rr[:, 0:N]), [r_rr], [r_rr])
                    for oc in range(5):
                        tb = oc % 2
                        rr = rq if oc < 3 else rkv
                        if oc < 3 and not isx:
                            continue
                        P.tt("dve", tmp[tb][:, 0:N], dsb[:, oc, 0:N], rr[:, 0:N], ALU.mult, [r_dsb, r_rr], [r_tmp[tb]])
                        if oc < 3:
                            P.actv(cqn[:, oc, t0:t0 + N], tmp[tb][:, 0:N], AF.Identity, [r_tmp[tb], r_gv], [r_cqn],
                                   scale=gv[:, oc:oc + 1])
                        else:
                            P.actv(ckvn[:, oc - 3, 0:N], tmp[tb][:, 0:N], AF.Identity, [r_tmp[tb], r_gv], [r_ckvn],
                                   scale=gv[:, oc:oc + 1])
                    if isx:
                        P.tt("dve", tmp[0][0:64, 0:N], dsb[0:64, 5, 0:N], rope[:, 0, 0:N], ALU.mult, [r_dsb, r_rope], [r_tmp[0]])
                        P.tt("pool", tmp[1][0:64, 0:N], dsw[0:64, 0:N], rope[:, 1, 0:N], ALU.mult, [r_dsw, r_rope], [r_tmp[1]])
                        P.tt("dve", KpT[0:64, t0:t0 + N], tmp[0][0:64, 0:N], tmp[1][0:64, 0:N], ALU.add,
                             [r_tmp[0], r_tmp[1]], [r_KpT])
                    else:
                        P.op("dve", lambda E, N=N, t0=t0: E.tensor_copy(out=KpT[0:64, t0:t0 + N], in_=dsb[0:64, 5, 0:N]),
                             [r_dsb], [r_KpT])
                    for h in range(8):
                        bg.step(2)
                        pb = m % 2
                        m += 1
                        for k in range(2):
                            P.mm(psk[pb][:, 0:N], wuk[:, k, h * 128:(h + 1) * 128], ckvn[:, k, 0:N], k == 0, k == 1,
                                 [r_wu, r_ckvn], [r_psk[pb]])
                        if h % 2 == 0:
                            P.op("act", lambda E, pb=pb, h=h, N=N, t0=t0: E.copy(out=KnT[:, h, t0:t0 + N], in_=psk[pb][:, 0:N]),
                                 [r_psk[pb]], [r_KnT])
                        else:
                            P.op("dve", lambda E, pb=pb, h=h, N=N, t0=t0: E.tensor_copy(out=KnT[:, h, t0:t0 + N], in_=psk[pb][:, 0:N]),
                                 [r_psk[pb]], [r_KnT])
                    for jt in range(N // 128):
                        for hf in range(2):
                            pb = m % 2
                            m += 1
                            for k in range(2):
                                P.mm(psk[pb][:, :], ckvn[:, k, jt * 128:(jt + 1) * 128], wuv[:, k, hf * 512:(hf + 1) * 512],
                                     k == 0, k == 1, [r_ckvn, r_wu], [r_psk[pb]])
                            kc = t0 // 128 + jt
                            if hf == 0:
                                P.op("act", lambda E, pb=pb, kc=kc: E.copy(out=V[:, kc, 0:512], in_=psk[pb][:, :]),
                                     [r_psk[pb]], [r_V])
                            else:
                                P.op("dve", lambda E, pb=pb, kc=kc: E.tensor_copy(out=V[:, kc, 512:1024], in_=psk[pb][:, :]),
                                     [r_psk[pb]], [r_V])
                bg.detach()
                P.barrier()
            with P.phase():
                wuq = P.sb("wuq", [128, 3, 1536], BF16); r_wq = Res()
                for k in range(3):
                    P.dma("pool", wuq[:, k, :], Dm["mla_w_uq_%d" % L][k * 128:(k + 1) * 128, :], W=[r_wq])
                wqsw = P.sb("wqsw", [128, 3, 512], BF16)
                for h in range(8):
                    P.op("pool", lambda E, h=h: E.tensor_copy(out=wqsw[:, :, h * 64:h * 64 + 32],
                                                             in_=wuq[:, :, h * 192 + 160:h * 192 + 192]), [r_wq], [r_wq])
                    P.op("pool", lambda E, h=h: E.tensor_copy(out=wqsw[:, :, h * 64 + 32:h * 64 + 64],
                                                             in_=wuq[:, :, h * 192 + 128:h * 192 + 160]), [r_wq], [r_wq])
                wo = P.sb("wo", [128, 8, 1024], BF16); r_wo = Res()
                for k in range(8):
                    P.dma("pool", wo[:, k, :], Dm["mla_w_o_%d" % L][k * 128:(k + 1) * 128, :], W=[r_wo])
                ones_b = P.sb("ones_b", [128, 128], BF16); r_on = Res()
                P.op("pool", lambda E: E.memset(ones_b[:], 1.0), W=[r_on])
                QnT = P.sb("QnT", [128, 8, 512], BF16); r_Qn = Res()
                QpT = P.sb("QpT", [128, 8, 512], BF16); r_Qp = Res()
                OT = P.sb("OT", [128, 8, 512], BF16); r_OT = Res()
                PT = [P.sb("PT", [128, 512], BF16) for _ in range(3)]; r_PT = [Res() for _ in range(3)]
                rope = P.sb("rope", [64, 2, 512]); r_rope = Res()
                sqb = [P.sb("sqb", [128, 512], BF16) for _ in range(2)]; r_sqb = [Res(), Res()]
                t1 = P.sb("t1", [128, 512]); t2 = P.sb("t2", [128, 512]); r_t = [Res(), Res()]
                rden = P.sb("rden", [128, 512]); r_rden = Res()
                kmx = P.sb("kmx", [128, 8, 8]); r_kmx = Res()
                negK = P.sb("negK", [128, 8]); r_negK = Res()
                pss = [P.ps("pss", [128, 512]) for _ in range(2)]; r_pss = [Res(), Res()]
                pso = P.ps("pso", [128, 512]); r_pso = Res()
                psden = P.ps("psden", [128, 512]); r_psden = Res()
                psy = [P.ps("psy", [128, 512]) for _ in range(2)]; r_psy = [Res(), Res()]
                psT = [P.ps("psT", [128, 512]) for _ in range(2)]; r_psT = [Res(), Res()]
                epi = Epi(P, Dm, C, layer, 1, nxt, True, src, dst, Dm["hB"], psT, r_psT, psden, r_psden, ns=2)
                P.op("pool", lambda E: E.memset(kmx[:], 0.0), W=[r_kmx])
                m = 0
                for h in range(8):
                    for ti, t0 in enumerate(range(0, NKV, 512)):
                        N = min(512, NKV - t0)
                        pb = m % 2
                        m += 1
                        P.actv(sqb[0][:, 0:N], KnT[:, h, t0:t0 + N], AF.Square, [r_KnT], [r_sqb[0]])
                        P.actv(sqb[1][0:64, 0:N], KpT[0:64, t0:t0 + N], AF.Square, [r_KpT], [r_sqb[1]])
                        P.mm(pss[pb][:, 0:N], ones_b[:], sqb[0][:, 0:N], True, False, [r_on, r_sqb[0]], [r_pss[pb]], sig=False)
                        P.mm(pss[pb][:, 0:N], ones_b[0:64, :], sqb[1][0:64, 0:N], False, True, [r_on, r_sqb[1]], [r_pss[pb]])
                        P.op("dve", lambda E, pb=pb, N=N, h=h, ti=ti: E.tensor_reduce(
                            out=kmx[:, h, ti:ti + 1], in_=pss[pb][:, 0:N], axis=AX.X, op=ALU.max), [r_pss[pb]], [r_kmx])
                P.op("dve", lambda E: E.tensor_reduce(out=negK[:], in_=kmx[:], axis=AX.X, op=ALU.max), [r_kmx], [r_negK])
                P.actv(negK[:], negK[:], AF.Sqrt, [r_negK], [r_negK])
                P.ts("dve", negK[:], negK[:], -1.02, None, ALU.mult, None, [r_negK], [r_negK])
                nonlocal_m = [m]
                nonlocal_p = [0]
                for qt in range(4):
                    q0 = qt * 512
                    m = nonlocal_m[0] + 1
                    P.dma("sp", rope[:], Dm["ropeCS"][:, :, q0:q0 + 512], W=[r_rope])
                    for h in range(8):
                        pb = m % 2
                        m += 1
                        for k in range(3):
                            P.mm(pss[pb][:, :], wuq[:, k, h * 192:h * 192 + 128], cqn[:, k, q0:q0 + 512], k == 0, k == 2,
                                 [r_wq, r_cqn], [r_pss[pb]])
                        P.op("act", lambda E, pb=pb, h=h: E.copy(out=QnT[:, h, :], in_=pss[pb][:, :]), [r_pss[pb]], [r_Qn])
                        pb2 = m % 2
                        m += 1
                        for k in range(3):
                            P.mm(pss[pb2][0:64, :], wuq[:, k, h * 192 + 128:h * 192 + 192], cqn[:, k, q0:q0 + 512],
                                 k == 0, k == 2, [r_wq, r_cqn], [r_pss[pb2]])
                        P.tt("dve", t1[0:64, :], pss[pb2][0:64, :], rope[:, 0, :], ALU.mult, [r_pss[pb2], r_rope], [r_t[0]])
                        pb3 = m % 2
                        m += 1
                        for k in range(3):
                            P.mm(pss[pb3][0:64, :], wqsw[:, k, h * 64:(h + 1) * 64], cqn[:, k, q0:q0 + 512],
                                 k == 0, k == 2, [r_wq, r_cqn], [r_pss[pb3]])
                        P.tt("dve", t2[0:64, :], pss[pb3][0:64, :], rope[:, 1, :], ALU.mult, [r_pss[pb3], r_rope], [r_t[1]])
                        P.tt("pool", QpT[0:64, h, :], t1[0:64, :], t2[0:64, :], ALU.add, [r_t[0], r_t[1]], [r_Qp])
                        P.actv(sqb[0][:, :], QnT[:, h, :], AF.Square, [r_Qn], [r_sqb[0]])
                        P.actv(sqb[1][0:64, :], QpT[0:64, h, :], AF.Square, [r_Qp], [r_sqb[1]])
                        pb4 = m % 2
                        m += 1
                        P.mm(pss[pb4][:, :], ones_b[:], sqb[0][:, :], True, False, [r_on, r_sqb[0]], [r_pss[pb4]], sig=False)
                        P.mm(pss[pb4][:, :], ones_b[0:64, :], sqb[1][0:64, :], False, True, [r_on, r_sqb[1]], [r_pss[pb4]])
                        P.actv(t1[64:65, :], pss[pb4][64:65, :], AF.Sqrt, [r_pss[pb4]], [r_t[0]])
                        P.ts("dve", QpT[64:65, h, :], t1[64:65, :], negK[64:65, h:h + 1], None, ALU.mult, None,
                             [r_t[0], r_negK], [r_Qp])
                    nonlocal_m[0] = m
                    for h in range(8):
                        def emit_S(kc, h=h):
                            nonlocal_m[0] += 1
                            pb = nonlocal_m[0] % 2
                            P.mm(pss[pb][:, :], KnT[:, h, kc * 128:(kc + 1) * 128], QnT[:, h, :], True, False,
                                 [r_KnT, r_Qn], [r_pss[pb]], sig=False)
                            P.mm(pss[pb][:, :], KpT[0:65, kc * 128:(kc + 1) * 128], QpT[0:65, h, :], False, True,
                                 [r_KpT, r_Qp], [r_pss[pb]])
                            pi = nonlocal_p[0] % 3
                            nonlocal_p[0] += 1
                            P.actv(PT[pi][:], pss[pb][:, :], AF.Exp, [r_pss[pb]], [r_PT[pi]], scale=ATTN_SCALE)
                            return pi
                        pis = {0: emit_S(0)}
                        for kc in range(NKC):
                            if kc + 1 < NKC:
                                pis[kc + 1] = emit_S(kc + 1)
                            pi = pis[kc]
                            P.mm(pso[:, :], V[:, kc, h * 128:(h + 1) * 128], PT[pi][:], kc == 0, kc == NKC - 1,
                                 [r_V, r_PT[pi]], [r_pso], sig=True)
                            P.mm(psden[:, :], ones_b[:], PT[pi][:], kc == 0, kc == NKC - 1,
                                 [r_on, r_PT[pi]], [r_psden], sig=True)
                        P.op("dve", lambda E: E.reciprocal(out=rden[:], in_=psden[:, :]), [r_psden], [r_rden])
                        P.tt("dve", OT[:, h, :], pso[:, :], rden[:], ALU.mult, [r_pso, r_rden], [r_OT])
                    for jt in range(4):
                        for hf in range(2):
                            for k in range(8):
                                P.mm(psy[hf][:, :], OT[:, k, jt * 128:(jt + 1) * 128], wo[:, k, hf * 512:(hf + 1) * 512],
                                     k == 0, k == 7, [r_OT, r_wo], [r_psy[hf]])
                        epi.tile(b * BSTR + q0 + jt * 128, [(psy[0][:, :], r_psy[0]), (psy[1][:, :], r_psy[1])])
                epi.flush()
                P.barrier()

def moe_supertiles(layer):
    runs = active_runs(layer)
    tiles = []
    for (s0, ln) in runs:
        tiles += list(range(s0, s0 + ln, 128))
    sts = []
    for i in range(0, len(tiles), 12):
        sts.append(tiles[i:i + 12])
    return sts


def phase_moe(P, Dm, C, layer, src, dst, nxt):
    for stiles in moe_supertiles(layer):
        nt = len(stiles)
        ngrp = nt // 4
        with P.phase():
            hT = P.sb("hT", [128, 8, nt * 128], BF16); r_hT = Res()
            gates = P.sb("gates", [128, nt, 64]); r_g = Res()
            i0 = 0
            while i0 < nt:
                i1 = i0
                while i1 + 1 < nt and stiles[i1 + 1] == stiles[i1] + 128:
                    i1 += 1
                ta, tb = stiles[i0], stiles[i1] + 128
                for k in range(8):
                    P.dma("sp" if k % 2 == 0 else "act", hT[:, k, i0 * 128:(i1 + 1) * 128], Dm["hB"][:, k, ta:tb],
                          W=[r_hT])
                P.dma("sp", gates[:, i0:i1 + 1, :], Dm["gates"][ta:tb, :].rearrange("(n p) e -> p n e", p=128), W=[r_g])
                i0 = i1 + 1
            yacc = P.sb("yacc", [128, nt, 1024]); r_y = [Res() for _ in range(nt)]
            NWB = 3
            w1 = [P.sb("w1", [128, 8, 256], BF16) for _ in range(NWB)]
            w3 = [P.sb("w3", [128, 8, 256], BF16) for _ in range(NWB)]
            w2 = [P.sb("w2", [128, 2, 1024], BF16) for _ in range(NWB)]
            r_w13 = [Res() for _ in range(NWB)]
            r_w2 = [Res() for _ in range(NWB)]
            gT = [P.sb("gT", [128, 2, 512], BF16) for _ in range(2)]; r_gT = [[Res(), Res()] for _ in range(2)]
            sg = [P.sb("sg", [128, 512]) for _ in range(2)]; r_sg = [Res(), Res()]
            psh = [[P.ps("psh", [128, 512]) for _ in range(2)] for _ in range(2)]
            r_psh = [[Res(), Res()] for _ in range(2)]
            psy = [P.ps("psy", [128, 512]) for _ in range(4)]; r_psy = [Res() for _ in range(4)]
            ytmp = [P.sb("ytmp", [128, 512]) for _ in range(4)]; r_ytmp = [Res() for _ in range(4)]
            POOL_SLOTS = (1, 4, 6)

            def load_w(e, wb):
                if e < 0:
                    a1, a3, a2 = Dm["sh_w1_%d" % layer], Dm["sh_w3_%d" % layer], Dm["sh_w2_%d" % layer]
                else:
                    a1, a3, a2 = Dm["moe_w1_%d" % layer][e], Dm["moe_w3_%d" % layer][e], Dm["moe_w2_%d" % layer][e]
                P.dma("pool", w1[wb][:], a1.rearrange("(k p) f -> p k f", p=128), W=[r_w13[wb]])
                P.dma("pool", w3[wb][:], a3.rearrange("(k p) f -> p k f", p=128), W=[r_w13[wb]])
                P.dma("pool", w2[wb][:], a2.rearrange("(k p) f -> p k f", p=128), W=[r_w2[wb]])

            units = [(e, g) for e in range(-1, 64) for g in range(ngrp)]
            ycnt = [0]

            def emit_H(u, ui):
                e, g = u
                wb = (e + 1) % NWB
                gb = ui % 2
                for c in range(2):
                    for (wt, pi) in ((w1, 0), (w3, 1)):
                        for k in range(8):
                            P.mm(psh[c][pi][:, :], wt[wb][:, k, c * 128:(c + 1) * 128], hT[:, k, g * 512:(g + 1) * 512],
                                 k == 0, k == 7, [r_w13[wb], r_hT], [r_psh[c][pi]])
                    P.actv(sg[c][:], psh[c][0][:, :], AF.Silu, [r_psh[c][0]], [r_sg[c]])
                    P.tt("dve", gT[gb][:, c, :], psh[c][1][:, :], sg[c][:], ALU.mult, [r_psh[c][1], r_sg[c]],
                         [r_gT[gb][c]])

            def emit_Y(u, ui):
                e, g = u
                wb = (e + 1) % NWB
                gb = ui % 2
                for jt in range(4):
                    ti = g * 4 + jt
                    for hf in range(2):
                        yb = ycnt[0] % 4
                        ycnt[0] += 1
                        for c in range(2):
                            P.mm(psy[yb][:, :], gT[gb][:, c, jt * 128:(jt + 1) * 128],
                                 w2[wb][:, c, hf * 512:(hf + 1) * 512], c == 0, c == 1,
                                 [r_gT[gb][c], r_w2[wb]], [r_psy[yb]])
                        ydst = yacc[:, ti, hf * 512:(hf + 1) * 512]
                        if e < 0:
                            P.op("act", lambda E, ydst=ydst, yb=yb: E.copy(out=ydst, in_=psy[yb][:, :]),
                                 [r_psy[yb]], [r_y[ti]])
                        else:
                            P.actv(ytmp[yb][:], psy[yb][:, :], AF.Copy, [r_psy[yb], r_g], [r_ytmp[yb]],
                                   scale=gates[:, ti, e:e + 1])
                            aeng = "pool" if (jt * 2 + hf) in POOL_SLOTS else "dve"
                            P.tt(aeng, ydst, ydst, ytmp[yb][:], ALU.add, [r_y[ti], r_ytmp[yb]], [r_y[ti]])

            loaded = set()

            def need_w(e):
                if e not in loaded and e < 64:
                    loaded.add(e)
                    load_w(e, (e + 1) % NWB)
            need_w(-1)
            need_w(0)
            for ui, u in enumerate(units):
                if ui == 0:
                    emit_H(u, ui)
                if ui + 1 < len(units):
                    nu = units[ui + 1]
                    need_w(nu[0])
                    emit_H(nu, ui + 1)
                emit_Y(u, ui)
                if u[1] == ngrp - 1:
                    need_w(u[0] + 2)
            epi = Epi(P, Dm, C, layer, 2, nxt, False, src, dst, Dm["hA"], [psh[0][0], psh[0][1]],
                      [r_psh[0][0], r_psh[0][1]])
            for ti, t0 in enumerate(stiles):
                epi.tile(t0, [(yacc[:, ti, 0:512], r_y[ti]), (yacc[:, ti, 512:1024], r_y[ti])])
            epi.flush()
            P.barrier()


IOA = bass.IndirectOffsetOnAxis
I32 = mybir.dt.int32
WROW = 6144


class BgW:
    def __init__(self, P, Dm, layer):
        self.P, self.Dm, self.layer = P, Dm, layer
        self.steps = [(e, part) for e in range(64) for part in range(3)]
        self.pos = 0
        self.pending = None
        self.bufs = None
        self.k = 0

    def attach(self, nbuf=3):
        self.bufs = [self.P.sb("bgw", [128, 2048], BF16) for _ in range(nbuf)]
        self.res = [Res() for _ in range(nbuf)]

    def _flush(self):
        if self.pending is not None:
            e, part, b = self.pending
            self.P.dma("sp", self.Dm["WS%d" % (self.layer % 2)][e * 128:(e + 1) * 128, part * 2048:(part + 1) * 2048],
                       self.bufs[b][:], R=[self.res[b]])
            self.pending = None

    def step(self, n=1):
        for _ in range(n):
            self._flush()
            if self.pos >= len(self.steps):
                return
            e, part = self.steps[self.pos]
            self.pos += 1
            b = self.k % len(self.bufs)
            self.k += 1
            L = self.layer
            if part == 0:
                src, kk = self.Dm["moe_w1_%d" % L][e], 8
            elif part == 1:
                src, kk = self.Dm["moe_w3_%d" % L][e], 8
            else:
                src, kk = self.Dm["moe_w2_%d" % L][e], 2
            self.P.dma("pool", self.bufs[b][:].rearrange("p (k f) -> p k f", k=kk),
                       src.rearrange("(k p) f -> p k f", p=128), W=[self.res[b]])
            self.pending = (e, part, b)

    def detach(self):
        self._flush()
        self.bufs = None

    def done(self):
        return self.pos >= len(self.steps) and self.pending is None


def moe_tiles(layer):
    return [t for (s0, ln) in active_runs(layer) for t in range(s0, s0 + ln, 128)]


def phase_wtable(P, Dm, C, layer, bg):
    if bg.done():
        return
    with P.phase():
        bg.attach(6)
        while not bg.done():
            bg.step(1)
        bg.detach()
        P.barrier()


def phase_dispatch(P, Dm, C, layer, bgn=None):
    tl = moe_tiles(layer)
    NT = len(tl)
    NB = NT * 8 + 64
    runs = active_runs(layer)
    with P.phase():
        r_c = Res()
        Ustr = P.sb("Ustr", [128, 128], BF16); onesb = P.sb("onesb", [128, 128], BF16)
        U64 = P.sb("U64", [64, 64]); UI64 = P.sb("UI64", [64, 64]); ones64 = P.sb("ones64", [64, 128])
        piota = P.sb("piota", [128, 1]); bstart = P.sb("bstart", [64, NB])
        for (t, npart, nfree, cmp_) in ((Ustr, 128, 128, ALU.is_gt), (U64, 64, 64, ALU.is_gt), (UI64, 64, 64, ALU.is_ge)):
            P.op("pool", lambda E, t=t: E.memset(t[:], 1.0), W=[r_c])
            P.op("pool", lambda E, t=t, nfree=nfree, cmp_=cmp_: E.affine_select(
                out=t[:], in_=t[:], pattern=[[1, nfree]], compare_op=cmp_, fill=0.0, base=0, channel_multiplier=-1),
                R=[r_c], W=[r_c])
        P.op("pool", lambda E: E.memset(onesb[:], 1.0), W=[r_c])
        P.op("pool", lambda E: E.memset(ones64[:], 1.0), W=[r_c])
        P.op("pool", lambda E: E.iota(piota[:], pattern=[[0, 1]], base=0, channel_multiplier=1,
                                      allow_small_or_imprecise_dtypes=True), W=[r_c])
        P.op("pool", lambda E: E.iota(bstart[:], pattern=[[128, NB]], base=0, channel_multiplier=0,
                                      allow_small_or_imprecise_dtypes=True), W=[r_c])
        if bgn is not None:
            bgn.attach(3)
        Gall = P.sb("Gall", [128, NT, 64]); r_G = Res()
        Mall = P.sb("Mall", [128, NT, 64], BF16); r_M = Res()
        Sall = P.sb("Sall", [128, NT, 64], BF16); r_S = Res()
        key = P.sb("key", [128, NT, 64]); r_key = [Res() for _ in range(NT)]
        d8k = P.sb("d8k", [128, NT, 8]); r_d8k = Res()
        g8 = P.sb("g8", [128, NT, 8]); r_g8 = Res()
        d8i = P.sb("d8i", [128, NT, 8], I32); r_d8i = Res()
        junk = [P.sb("junk", [128, 64]) for _ in range(2)]; r_junk = [Res(), Res()]
        pcnt = P.ps("pcnt", [128, 512]); r_pcnt = Res()
        ppos = [P.ps("ppos", [128, 512]) for _ in range(2)]; r_ppos = [Res(), Res()]
        pmisc = P.ps("pmisc", [128, 512]); r_pmisc = Res()
        n0 = 0
        for (s0, ln) in runs:
            k = ln // 128
            P.dma("sp", Gall[:, n0:n0 + k, :], Dm["gates"][s0:s0 + ln, :].rearrange("(n p) e -> p n e", p=128), W=[r_G])
            n0 += k
        fl = lambda t: t[:].rearrange("p n e -> p (n e)")
        P.op("dve", lambda E: E.tensor_single_scalar(out=fl(Mall), in_=fl(Gall), scalar=0.0, op=ALU.is_gt), [r_G], [r_M])
        P.op("pool", lambda E: E.memset(Sall[:, 0, :], 0.0), W=[r_S])
        P.op("pool", lambda E: E.memset(fl(g8), 0.0), W=[r_g8])
        for n in range(1, NT):
            P.tt("dve", Sall[:, n, :], Sall[:, n - 1, :], Mall[:, n - 1, :], ALU.add, [r_S, r_M], [r_S])
        for n in range(NT):
            P.mm(pcnt[0:64, 0:2], Mall[:, n, :], onesb[:, 0:2], n == 0, n == NT - 1, [r_M, r_c], [r_pcnt])
        cnt = P.sb("cnt", [64, 2]); rr = P.sb("rr", [64, 2]); pad = P.sb("pad", [64, 2]); r_pad = Res()
        padB = P.sb("padB", [64, 128]); pend = P.sb("pend", [64, 2]); ps1 = P.sb("ps1", [128, 64]); r_ps1 = Res()
        MAGIC = 12582912.0
        P.ts("dve", cnt[:], pcnt[0:64, 0:2], 1.0 / 128.0, 127.0 / 256.0, ALU.mult, ALU.add, [r_pcnt], [r_pad])
        P.ts("dve", rr[:], cnt[:], MAGIC, None, ALU.add, None, [r_pad], [r_pad])
        P.ts("dve", pad[:], rr[:], -MAGIC, 128.0, ALU.add, ALU.mult, [r_pad], [r_pad])
        P.ts("dve", padB[:], ones64[:], pad[:, 0:1], None, ALU.mult, None, [r_c, r_pad], [r_pad])
        P.mm(pmisc[:, 0:64], padB[:], U64[:], True, True, [r_pad, r_c], [r_pmisc])
        P.ts("dve", ps1[:], pmisc[:, 0:64], 1.0, None, ALU.add, None, [r_pmisc], [r_ps1])
        P.mm(pmisc[0:64, 64:66], UI64[:], pad[:], True, True, [r_pad, r_c], [r_pmisc])
        P.op("dve", lambda E: E.tensor_copy(out=pend[:], in_=pmisc[0:64, 64:66]), [r_pmisc], [r_pad])
        cmpT = P.sb("cmpT", [64, NB], BF16); idxf = P.sb("idxf", [128, NB]); idxw = P.sb("idxw", [128, NB], I32); r_ix = Res()
        P.ts("dve", cmpT[:], bstart[:], pend[:, 0:1], None, ALU.is_ge, None, [r_c, r_pad], [r_ix])
        P.mm(pmisc[:, 0:NB], onesb[0:64, :], cmpT[:], True, True, [r_c, r_ix], [r_pmisc])
        P.ts("dve", idxf[:], pmisc[:, 0:NB], 63.0, 128.0, ALU.min, ALU.mult, [r_pmisc], [r_ix])
        P.ts("dve", idxw[:], idxf[:], piota[:, 0:1], None, ALU.add, None, [r_ix, r_c], [r_ix])
        P.dma("sp", Dm["idxw"][:, 0:NB], idxw[:], R=[r_ix])
        for n in range(NT):
            if bgn is not None:
                bgn.step(2)
            pb = n % 2
            P.mm(ppos[pb][:, 0:64], Ustr[:], Mall[:, n, :], True, False, [r_c, r_M], [r_ppos[pb]], sig=False)
            P.mm(ppos[pb][:, 0:64], onesb[:], Sall[:, n, :], False, True, [r_c, r_S], [r_ppos[pb]])
            P.tt("dve", key[:, n, :], ppos[pb][:, 0:64], ps1[:], ALU.add, [r_ppos[pb], r_ps1], [r_key[n]])
            P.tt("dve", key[:, n, :], key[:, n, :], Mall[:, n, :], ALU.mult, [r_key[n], r_M], [r_key[n]])
            P.op("dve", lambda E, n=n: E.max(out=d8k[:, n, :], in_=key[:, n, :]), [r_key[n]], [r_d8k])
            for j in range(8):
                jb = j % 2
                P.op("dve", lambda E, n=n, j=j, jb=jb: E.scalar_tensor_tensor(
                    out=junk[jb][:], in0=key[:, n, :], scalar=d8k[:, n, j:j + 1], in1=Gall[:, n, :],
                    op0=ALU.is_equal, op1=ALU.mult, accum_out=g8[:, n, j:j + 1]),
                    [r_key[n], r_d8k, r_G], [r_junk[jb], r_g8])
        P.ts("dve", d8i[:].rearrange("p n j -> p (n j)"), d8k[:].rearrange("p n j -> p (n j)"), -1.0, None, ALU.add, None,
             [r_d8k], [r_d8i])
        n0 = 0
        for (s0, ln) in runs:
            k = ln // 128
            P.dma("sp", Dm["d8i"][s0:s0 + ln, :].rearrange("(n p) j -> p n j", p=128), d8i[:, n0:n0 + k, :], R=[r_d8i])
            P.dma("sp", Dm["g8"][s0:s0 + ln, :].rearrange("(n p) j -> p n j", p=128), g8[:, n0:n0 + k, :], R=[r_g8])
            n0 += k
        hrt = [P.sb("hrt", [128, 1024], BF16) for _ in range(3)]; r_hrt = [Res() for _ in range(3)]
        r_hbuf = Res()
        for n, t0 in enumerate(tl):
            b = n % 3
            P.dma("sp", hrt[b][:], Dm["hrow"][t0:t0 + 128, :], W=[r_hrt[b]])
            for j in range(8):
                P.idma(Dm["hbuf"], IOA(d8i[:, n, j:j + 1], 0), hrt[b][:], None, R=[r_hrt[b], r_d8i], W=[])
        if bgn is not None:
            bgn.detach()
        P.barrier()
    return NB


def phase_blocks(P, Dm, C, layer, NB):
    with P.phase():
        idxw = P.sb("idxw", [128, NB], I32); r_ix = Res()
        P.dma("sp", idxw[:], Dm["idxw"][:, 0:NB], W=[r_ix])
        NWB = 6
        NHB = 6
        wb = [P.sb("wb", [128, WROW], BF16) for _ in range(NWB)]; r_wb = [Res() for _ in range(NWB)]
        hblk = [P.sb("hblk", [128, 1024], BF16) for _ in range(NHB)]; r_hblk = [Res() for _ in range(NHB)]
        hTk = [P.sb("hTk", [128, 8, 128], BF16) for _ in range(2)]; r_hTk = [Res(), Res()]
        sg = [P.sb("sg", [128, 256]) for _ in range(2)]; r_sg = [Res(), Res()]
        gTb = [P.sb("gTb", [128, 256], BF16) for _ in range(2)]; r_gTb = [Res(), Res()]
        ysb = [P.sb("ysb", [128, 1024]) for _ in range(4)]; r_ysb = [Res() for _ in range(4)]
        psT = [P.ps("psTb", [128, 1024], BF16) for _ in range(2)]; r_psT = [Res(), Res()]
        ph = [P.ps("ph", [128, 512]) for _ in range(2)]; r_ph = [Res(), Res()]
        psy = [P.ps("psy", [128, 512]) for _ in range(4)]; r_psy = [Res() for _ in range(4)]

        def load(b):
            P.idma(wb[b % NWB][:], None, Dm["WS%d" % (layer % 2)], IOA(idxw[:, b:b + 1], 0), R=[r_ix], W=[r_wb[b % NWB]])
            P.dma("sp", hblk[b % NHB][:], Dm["hbuf"][b * 128:(b + 1) * 128, :], W=[r_hblk[b % NHB]])

        def emit_TH(b):
            i2 = b % 2
            w = wb[b % NWB]
            hb = hblk[b % NHB]
            for k in range(8):
                P.op("pe", lambda E, k=k: E.transpose(out=psT[i2][:, k * 128:(k + 1) * 128], in_=hb[:, k * 128:(k + 1) * 128],
                                                      identity=C.identb[:]),
                     [r_hblk[b % NHB], C.r_const], [r_psT[i2]], sig=(k == 7))
            P.op("act", lambda E: E.copy(out=hTk[i2][:, 0:4, :], in_=psT[i2][:, 0:512].rearrange("p (k t) -> p k t", k=4)),
                 [r_psT[i2]], [r_hTk[i2]])
            P.op("dve", lambda E: E.tensor_copy(out=hTk[i2][:, 4:8, :], in_=psT[i2][:, 512:1024].rearrange("p (k t) -> p k t", k=4)),
                 [r_psT[i2]], [r_hTk[i2]])
            for wi in range(2):
                for c in range(2):
                    for k in range(8):
                        col = wi * 2048 + k * 256 + c * 128
                        P.mm(ph[i2][:, wi * 256 + c * 128:wi * 256 + (c + 1) * 128], w[:, col:col + 128], hTk[i2][:, k, :],
                             k == 0, k == 7, [r_wb[b % NWB], r_hTk[i2]], [r_ph[i2]], sig=(k == 7 and c == 1))
            P.actv(sg[i2][:], ph[i2][:, 0:256], AF.Silu, [r_ph[i2]], [r_sg[i2]])
            P.tt("dve", gTb[i2][:], ph[i2][:, 256:512], sg[i2][:], ALU.mult, [r_ph[i2], r_sg[i2]], [r_gTb[i2]])

        def emit_Y(b):
            i2 = b % 2
            w = wb[b % NWB]
            for hf in range(2):
                yb = (b % 2) * 2 + hf
                for c in range(2):
                    col = 4096 + c * 1024 + hf * 512
                    P.mm(psy[yb][:, :], gTb[i2][:, c * 128:(c + 1) * 128], w[:, col:col + 512], c == 0, c == 1,
                         [r_gTb[i2], r_wb[b % NWB]], [r_psy[yb]])
                i4 = b % 4
                if hf == 0:
                    P.op("act", lambda E, yb=yb, i4=i4: E.copy(out=ysb[i4][:, 0:512], in_=psy[yb][:, :]), [r_psy[yb]], [r_ysb[i4]])
                else:
                    P.op("dve", lambda E, yb=yb, i4=i4: E.tensor_copy(out=ysb[i4][:, 512:1024], in_=psy[yb][:, :]),
                         [r_psy[yb]], [r_ysb[i4]])
            P.dma("act", Dm["ybuf"][b * 128:(b + 1) * 128, :], ysb[b % 4][:], R=[r_ysb[b % 4]])

        PF = 5
        for b in range(PF):
            load(b)
        emit_TH(0)
        for b in range(NB):
            if b + PF < NB:
                load(b + PF)
            if b + 1 < NB:
                emit_TH(b + 1)
            emit_Y(b)
        P.barrier()


def phase_combine(P, Dm, C, layer, src, dst, nxt, bgn=None):
    for stiles in moe_supertiles(layer):
        nt = len(stiles)
        ngrp = nt // 4
        with P.phase():
            hT = P.sb("hT", [128, 8, nt * 128], BF16); r_hT = Res()
            d8i = P.sb("d8i", [128, nt, 8], I32); g8 = P.sb("g8", [128, nt, 8]); r_g = Res()
            i0 = 0
            while i0 < nt:
                i1 = i0
                while i1 + 1 < nt and stiles[i1 + 1] == stiles[i1] + 128:
                    i1 += 1
                ta, tb = stiles[i0], stiles[i1] + 128
                for k in range(8):
                    P.dma("sp" if k % 2 == 0 else "act", hT[:, k, i0 * 128:(i1 + 1) * 128], Dm["hB"][:, k, ta:tb], W=[r_hT])
                P.dma("sp", d8i[:, i0:i1 + 1, :], Dm["d8i"][ta:tb, :].rearrange("(n p) j -> p n j", p=128), W=[r_g])
                P.dma("sp", g8[:, i0:i1 + 1, :], Dm["g8"][ta:tb, :].rearrange("(n p) j -> p n j", p=128), W=[r_g])
                i0 = i1 + 1
            if bgn is not None:
                bgn.attach(3)
            yacc = P.sb("yacc", [128, nt, 1024]); r_y = [Res() for _ in range(nt)]
            w1 = P.sb("w1", [128, 8, 256], BF16); w3 = P.sb("w3", [128, 8, 256], BF16); w2 = P.sb("w2", [128, 2, 1024], BF16)
            r_w = Res()
            P.dma("pool", w1[:], Dm["sh_w1_%d" % layer].rearrange("(k p) f -> p k f", p=128), W=[r_w])
            P.dma("pool", w3[:], Dm["sh_w3_%d" % layer].rearrange("(k p) f -> p k f", p=128), W=[r_w])
            P.dma("pool", w2[:], Dm["sh_w2_%d" % layer].rearrange("(k p) f -> p k f", p=128), W=[r_w])
            gT = [P.sb("gT", [128, 2, 512], BF16) for _ in range(2)]; r_gT = [Res(), Res()]
            sg = [P.sb("sg", [128, 512]) for _ in range(2)]; r_sg = [Res(), Res()]
            NYG = 8
            yg = [P.sb("yg", [128, 1024]) for _ in range(NYG)]; r_yg = [Res() for _ in range(NYG)]
            psh = [[P.ps("psh", [128, 512]) for _ in range(2)] for _ in range(2)]
            r_psh = [[Res(), Res()] for _ in range(2)]
            psy = [P.ps("psy", [128, 512]) for _ in range(4)]; r_psy = [Res() for _ in range(4)]
            ycnt = 0
            for g in range(ngrp):
                gb = g % 2
                for c in range(2):
                    for (wt, pi) in ((w1, 0), (w3, 1)):
                        for k in range(8):
                            P.mm(psh[c][pi][:, :], wt[:, k, c * 128:(c + 1) * 128], hT[:, k, g * 512:(g + 1) * 512],
                                 k == 0, k == 7, [r_w, r_hT], [r_psh[c][pi]])
                    P.actv(sg[c][:], psh[c][0][:, :], AF.Silu, [r_psh[c][0]], [r_sg[c]])
                    P.tt("dve", gT[gb][:, c, :], psh[c][1][:, :], sg[c][:], ALU.mult, [r_psh[c][1], r_sg[c]], [r_gT[gb]])
                for jt in range(4):
                    ti = g * 4 + jt
                    for hf in range(2):
                        yb = ycnt % 4
                        ycnt += 1
                        for c in range(2):
                            P.mm(psy[yb][:, :], gT[gb][:, c, jt * 128:(jt + 1) * 128], w2[:, c, hf * 512:(hf + 1) * 512],
                                 c == 0, c == 1, [r_gT[gb], r_w], [r_psy[yb]])
                        ydst = yacc[:, ti, hf * 512:(hf + 1) * 512]
                        if hf == 0:
                            P.op("act", lambda E, ydst=ydst, yb=yb: E.copy(out=ydst, in_=psy[yb][:, :]), [r_psy[yb]], [r_y[ti]])
                        else:
                            P.op("dve", lambda E, ydst=ydst, yb=yb: E.tensor_copy(out=ydst, in_=psy[yb][:, :]),
                                 [r_psy[yb]], [r_y[ti]])
            items = [(ti, j) for ti in range(nt) for j in range(8)]
            epi = Epi(P, Dm, C, layer, 2, nxt, False, src, dst, Dm["hA"], [psh[0][0], psh[0][1]],
                      [r_psh[0][0], r_psh[0][1]])

            def gather(ix):
                ti, j = items[ix]
                P.idma(yg[ix % NYG][:], None, Dm["ybuf"], IOA(d8i[:, ti, j:j + 1], 0), R=[r_g], W=[r_yg[ix % NYG]])
            for ix in range(min(NYG - 1, len(items))):
                gather(ix)
            for ix, (ti, j) in enumerate(items):
                if bgn is not None and ix % 2 == 0:
                    bgn.step(1)
                if ix + NYG - 1 < len(items):
                    gather(ix + NYG - 1)
                yb = ix % NYG
                P.op("dve", lambda E, ti=ti, j=j, yb=yb: E.scalar_tensor_tensor(
                    out=yacc[:, ti, :], in0=yg[yb][:], scalar=g8[:, ti, j:j + 1], in1=yacc[:, ti, :],
                    op0=ALU.mult, op1=ALU.add), [r_yg[yb], r_g, r_y[ti]], [r_y[ti]])
                if j == 7:
                    epi.tile(stiles[ti], [(yacc[:, ti, 0:512], r_y[ti]), (yacc[:, ti, 512:1024], r_y[ti])])
                    if ti % 4 == 3:
                        epi.flush()
            epi.flush()
            if bgn is not None:
                bgn.detach()
            P.barrier()


def phase_moe_sparse(P, Dm, C, layer, src, dst, nxt, bg, bgn=None):
    phase_wtable(P, Dm, C, layer, bg)
    NB = phase_dispatch(P, Dm, C, layer, bgn)
    phase_blocks(P, Dm, C, layer, NB)
    phase_combine(P, Dm, C, layer, src, dst, nxt, bgn)


def build(layers, first_src="xin"):
    nc = bass.Bass("TRN2", target_bir_lowering=False)
    Dm = {}

    def din(name, shape, dt=F32):
        Dm[name] = nc.dram_tensor(name, list(shape), dt, kind="ExternalInput").ap()

    def dint(name, shape, dt=F32):
        Dm[name] = nc.dram_tensor(name, list(shape), dt, kind="Internal").ap()

    din("xin", [NTOK, 1024])
    din("ccT", [128, 32])
    for i in layers:
        kind = i % 3
        din("ada_w%d" % i, [1024, 6144]); din("ada_b%d" % i, [1, 6144])
        din("ln_g%d" % i, [2, 1024]); din("ln_b%d" % i, [2, 1024])
        if kind == 0:
            din("conf_w1_%d" % i, [1024, 2048]); din("conf_b1T_%d" % i, [128, 16])
            din("conf_dwT_%d" % i, [128, 8 * 31]); din("conf_pvec_%d" % i, [128, 24])
            din("conf_w2_%d" % i, [1024, 1024]); din("conf_b2_%d" % i, [1, 1024])
        elif kind == 1:
            din("sc_w_in_%d" % i, [1024, 3072]); din("sc_dwT_%d" % i, [128, 8 * 3]); din("sc_w_out_%d" % i, [1024, 1024])
        else:
            din("mla_w_dqkv_%d" % i, [1024, 704]); din("mla_gT_%d" % i, [128, 5])
            din("mla_w_uq_%d" % i, [384, 1536]); din("mla_w_uk_%d" % i, [256, 1024]); din("mla_w_uv_%d" % i, [256, 1024])
            din("mla_w_o_%d" % i, [1024, 1024])
            if "ropeCS" not in Dm:
                din("ropeCS", [64, 2, SEQ])
        din("moe_router%d" % i, [1024, 64]); din("moe_bias%d" % i, [1, 64])
        din("moe_w1_%d" % i, [64, 1024, 256]); din("moe_w3_%d" % i, [64, 1024, 256]); din("moe_w2_%d" % i, [64, 256, 1024])
        din("sh_w1_%d" % i, [1024, 256]); din("sh_w3_%d" % i, [1024, 256]); din("sh_w2_%d" % i, [256, 1024])
        dint("modrow%d" % i, [4, 6144])
    Dm["xout"] = nc.dram_tensor("xout", [NTOK, 1024], F32, kind="ExternalOutput").ap()
    dint("xresA", [NTOK, 1024]); dint("xresB", [NTOK, 1024])
    dint("hA", [128, 8, NTOK], BF16); dint("hB", [128, 8, NTOK], BF16)
    dint("gates", [NTOK, 64])
    dint("hrow", [NTOK, 1024], BF16)
    NSLOT = (36 * 8 + 64) * 128
    dint("hbuf", [NSLOT, 1024], BF16); dint("ybuf", [NSLOT, 1024]); dint("WS0", [64 * 128, WROW], BF16); dint("WS1", [64 * 128, WROW], BF16)
    dint("idxw", [128, 36 * 8 + 64], I32); dint("d8i", [NTOK, 8], I32); dint("g8", [NTOK, 8])

    P = Prog(nc)
    C = Consts()
    with P.gstack:
        phase_consts(P, C)
        phase_mod(P, Dm, C, layers)
        phase_prologue(P, Dm, C, layers[0])
        src = Dm["xin"]
        bgs = {i: BgW(P, Dm, i) for i in layers}
        for li, i in enumerate(layers):
            lastl = li == len(layers) - 1
            kind = i % 3
            P.new_epoch()
            bg = bgs[i]
            if kind in (0, 1):
                phase_convmix(P, Dm, C, i, src, Dm["xresA"], (i, 2), bg)
            else:
                phase_mla(P, Dm, C, i, src, Dm["xresA"], (i, 2), bg)
            P.new_epoch()
            nxt = None if lastl else (layers[li + 1], 1)
            bgn = None if lastl else bgs[layers[li + 1]]
            phase_moe_sparse(P, Dm, C, i, Dm["xresA"], Dm["xout"] if lastl else Dm["xresB"], nxt, bg, bgn)
            src = Dm["xresB"]
        if not has_ctx(layers[-1]):
            with P.phase():
                cp = P.sb("cp", [128, 2, 1024]); r_cp = Res()
                for b in range(2):
                    t0 = b * BSTR + SEQ
                    P.dma("sp", cp[:], src_last(Dm, layers)[t0:t0 + 256, :].rearrange("(n p) d -> p n d", p=128), W=[r_cp])
                    P.dma("sp", Dm["xout"][t0:t0 + 256, :].rearrange("(n p) d -> p n d", p=128), cp[:], R=[r_cp])
        P.barrier()
    return nc


def src_last(Dm, layers):
    cands = [i for i in layers if has_ctx(i)]
    if not cands:
        return Dm["xin"]
    return Dm["xresB"] if cands[-1] != layers[-1] else Dm["xout"]


def host_inputs(inputs, layers, xin_cores):
    f = lambda a: np.ascontiguousarray(np.asarray(a, dtype=np.float32))
    shared = {}
    for i in layers:
        kind, j = i % 3, i // 3
        shared["ada_w%d" % i] = f(inputs["ada_w"][i]); shared["ada_b%d" % i] = f(inputs["ada_b"][i][None, :])
        shared["ln_g%d" % i] = f(inputs["ln_g"][i]); shared["ln_b%d" % i] = f(inputs["ln_b"][i])
        if kind == 0:
            shared["conf_w1_%d" % i] = f(inputs["conf_w1"][j])
            shared["conf_b1T_%d" % i] = f(np.asarray(inputs["conf_b1"][j]).reshape(16, 128).T)
            shared["conf_dwT_%d" % i] = f(np.asarray(inputs["conf_dw"][j]).reshape(31, 8, 128).transpose(2, 1, 0).reshape(128, 248))
            pv = np.stack([np.asarray(inputs["conf_dwb"][j]).reshape(8, 128).T,
                           np.asarray(inputs["conf_ng"][j]).reshape(8, 128).T,
                           np.asarray(inputs["conf_nb"][j]).reshape(8, 128).T], axis=1)
            shared["conf_pvec_%d" % i] = f(pv.reshape(128, 24))
            shared["conf_w2_%d" % i] = f(inputs["conf_w2"][j]); shared["conf_b2_%d" % i] = f(inputs["conf_b2"][j][None, :])
        elif kind == 1:
            shared["sc_w_in_%d" % i] = f(inputs["sc_w_in"][j])
            shared["sc_dwT_%d" % i] = f(np.asarray(inputs["sc_dw"][j]).reshape(3, 8, 128).transpose(2, 1, 0).reshape(128, 24))
            shared["sc_w_out_%d" % i] = f(inputs["sc_w_out"][j])
        else:
            shared["mla_w_dqkv_%d" % i] = f(inputs["mla_w_dqkv"][j])
            gT = np.concatenate([np.asarray(inputs["mla_q_g"][j]).reshape(3, 128).T,
                                 np.asarray(inputs["mla_kv_g"][j]).reshape(2, 128).T], axis=1)
            shared["mla_gT_%d" % i] = f(gT)
            shared["mla_w_uq_%d" % i] = f(inputs["mla_w_uq"][j]); shared["mla_w_uk_%d" % i] = f(inputs["mla_w_uk"][j])
            shared["mla_w_uv_%d" % i] = f(inputs["mla_w_uv"][j]); shared["mla_w_o_%d" % i] = f(inputs["mla_w_o"][j])
            shared["ropeCS"] = rope_tables()
        shared["moe_router%d" % i] = f(inputs["moe_router"][i]); shared["moe_bias%d" % i] = f(inputs["moe_bias"][i][None, :])
        shared["moe_w1_%d" % i] = f(inputs["moe_w1"][i]); shared["moe_w3_%d" % i] = f(inputs["moe_w3"][i])
        shared["moe_w2_%d" % i] = f(inputs["moe_w2"][i])
        shared["sh_w1_%d" % i] = f(inputs["sh_w1"][i]); shared["sh_w3_%d" % i] = f(inputs["sh_w3"][i])
        shared["sh_w2_%d" % i] = f(inputs["sh_w2"][i])
    maps = []
    c = np.asarray(inputs["c"], dtype=np.float32)
    cctx = np.asarray(inputs["c_ctx"], dtype=np.float32)
    for core in range(NCORES):
        cc = np.zeros((4, 1024), np.float32)
        cc[0] = c[2 * core]; cc[1] = c[2 * core + 1]; cc[2] = cctx
        ccT = cc.reshape(4, 8, 128).transpose(2, 1, 0).reshape(128, 32)
        m = dict(shared)
        m["ccT"] = f(ccT)
        m["xin"] = xin_cores[core]
        maps.append(m)
    return maps


def rope_tables():
    n_freq = 16
    inv_freq = (10000.0 ** (-np.arange(n_freq, dtype=np.float32) / n_freq)).astype(np.float32)
    t = np.arange(SEQ)
    r = (t // 64).astype(np.float32)
    col = (t % 64).astype(np.float32)
    ang = np.concatenate([r[:, None] * inv_freq, col[:, None] * inv_freq], -1).astype(np.float32)
    cos = np.cos(ang).astype(np.float32).T
    sin = np.sin(ang).astype(np.float32).T
    out = np.empty((64, 2, SEQ), np.float32)
    out[0:32, 0] = cos; out[32:64, 0] = cos
    out[0:32, 1] = -sin; out[32:64, 1] = sin
    return np.ascontiguousarray(out)


def pack_x(inputs):
    x = np.asarray(inputs["x"], dtype=np.float32)
    ctx = np.asarray(inputs["ctx"], dtype=np.float32)
    out = []
    for core in range(NCORES):
        xin = np.empty((NTOK, 1024), np.float32)
        for b in range(2):
            xin[b * BSTR:b * BSTR + SEQ] = x[2 * core + b]
            xin[b * BSTR + SEQ:(b + 1) * BSTR] = ctx[2 * core + b]
        out.append(xin)
    return out


LAUNCH_GROUPS = [[0, 1, 2, 3]]
_NC_CACHE = {}


def run_groups(inputs, groups, core_ids=None):
    xin = pack_x(inputs)
    for layers in groups:
        key = tuple(layers)
        if key not in _NC_CACHE:
            _NC_CACHE[key] = build(layers)
        nc = _NC_CACHE[key]
        maps = host_inputs(inputs, layers, xin)
        if core_ids is not None:
            maps = [maps[c] for c in core_ids]
        res = run_bass_kernel_spmd(nc, maps, core_ids=list(range(len(maps))))
        outs = [np.asarray(r["xout"]) for r in res.results]
        if core_ids is not None:
            for k, c in enumerate(core_ids):
                xin[c] = outs[k]
        else:
            xin = outs
    return xin


def kernel(**inputs):
    xs = run_groups(inputs, LAUNCH_GROUPS)
    out = np.empty((16, SEQ, 1024), np.float32)
    for core in range(NCORES):
        for b in range(2):
            out[2 * core + b] = xs[core][b * BSTR:b * BSTR + SEQ]
    return out
```
